# Optimizing a Trainium2 kernel written in Bass

```python
import math
import jax, jax.numpy as jnp
from jax import lax
import numpy as np

D_MODEL = 1024
BATCH = 8
SEQ = 4096
DEPTH = 1

CHUNK = 64
EPS = 1e-6

DA_HEADS = 8
DA_DQK = 64
DA_DV = 2 * DA_DQK
DA_WIDTH = DA_HEADS * DA_DV
Q_BLOCK = 128

SG_BLOCK = 128
SG_GROUPS = 8
SG_WIDTH = D_MODEL
SG_GROUP_DIM = SG_WIDTH // SG_GROUPS

SPLIT_SIZES = (2 * DA_HEADS * DA_DQK,
               2 * DA_HEADS * DA_DQK,
               DA_WIDTH,
               SG_WIDTH,
               SG_WIDTH,
               2 * D_MODEL)
D_IN = sum(SPLIT_SIZES)

PEER_HEADS = 8
N_KEYS = 128
N_EXPERTS = N_KEYS * N_KEYS
PEER_TOPK = 16
D_KEY = 256
D_KEY_HALF = D_KEY // 2
PEER_TOKEN_BLOCK = 128

kernel_name = "hybrid_diffattn_gmlp_peer_block"


def rmsnorm(x, g):
    xf = x.astype(jnp.float32)
    y = xf * lax.rsqrt(jnp.mean(xf * xf, axis=-1, keepdims=True) + EPS)
    return (y * g).astype(x.dtype)


def layernorm(x, g, b):
    xf = x.astype(jnp.float32)
    mu = jnp.mean(xf, axis=-1, keepdims=True)
    var = jnp.mean(jnp.square(xf - mu), axis=-1, keepdims=True)
    y = (xf - mu) * lax.rsqrt(var + EPS)
    return (y * g + b).astype(x.dtype)


def lambda_init_for(layer):
    return 0.8 - 0.6 * math.exp(-0.3 * layer)


def diff_attention(q, k, v, lam, subln_g, lambda_init):
    B, S = q.shape[0], q.shape[1]
    nb = S // Q_BLOCK
    scale = DA_DQK ** -0.5
    qb = q.reshape(B, nb, Q_BLOCK, 2, DA_HEADS, DA_DQK).transpose(1, 0, 2, 3, 4, 5)
    key_chunk = jnp.arange(S) // CHUNK

    def block(args):
        qblk, bi = args
        q_chunk = (bi * Q_BLOCK + jnp.arange(Q_BLOCK)) // CHUNK
        allowed = key_chunk[None, :] <= q_chunk[:, None]
        s = jnp.einsum('bqmhd,bkmhd->bmhqk', qblk, k).astype(jnp.float32) * scale
        s = jnp.where(allowed, s, -1e30)
        p = jax.nn.softmax(s, axis=-1)
        a = p[:, 0] - lam * p[:, 1]
        return jnp.einsum('bhqk,bkhd->bqhd', a.astype(v.dtype), v)

    o = lax.map(block, (qb, jnp.arange(nb)))
    o = o.transpose(1, 0, 2, 3, 4).reshape(B, S, DA_HEADS, DA_DV)
    o = rmsnorm(o, subln_g) * (1.0 - lambda_init)
    return o.reshape(B, S, DA_WIDTH)


def spatial_gating(u, v, ln_g, ln_b, w_s, b_s):
    v = layernorm(v, ln_g, ln_b)
    B, S = v.shape[0], v.shape[1]
    nb = S // SG_BLOCK
    vb = v.reshape(B, nb, SG_BLOCK, SG_GROUPS, SG_GROUP_DIM)
    pos_chunk = jnp.arange(SG_BLOCK) // CHUNK
    mask = pos_chunk[:, None] >= pos_chunk[None, :]
    w = jnp.where(mask[None], w_s, 0)
    mixed = jnp.einsum('gij,bnjgc->bnigc', w, vb) + b_s.T[None, None, :, :, None]
    return u * mixed.reshape(B, S, SG_WIDTH)


def peer(x, w_query, subkeys, u_tab, v_tab):
    B, S, D = x.shape
    T = B * S
    xt = x.reshape(T, D)
    q = (xt @ w_query).reshape(T, PEER_HEADS, 2, D_KEY_HALF)
    s = jnp.einsum('thpc,pnc->thpn', q, subkeys).astype(jnp.float32)
    sv, si = lax.top_k(s, PEER_TOPK)
    cand = sv[:, :, 0, :, None] + sv[:, :, 1, None, :]
    cand_idx = si[:, :, 0, :, None] * N_KEYS + si[:, :, 1, None, :]
    cand = cand.reshape(T, PEER_HEADS, PEER_TOPK * PEER_TOPK)
    cand_idx = cand_idx.reshape(T, PEER_HEADS, PEER_TOPK * PEER_TOPK)
    top_s, pos = lax.top_k(cand, PEER_TOPK)
    experts = jnp.take_along_axis(cand_idx, pos, axis=-1)
    g = jax.nn.softmax(top_s, axis=-1)
    nb = T // PEER_TOKEN_BLOCK

    def block(args):
        xb, eb, gb = args
        u = u_tab[eb]
        hdn = jax.nn.gelu(jnp.einsum('td,thkd->thk', xb, u), approximate=False)
        wgt = gb.astype(xb.dtype) * hdn
        return jnp.einsum('thk,thkd->td', wgt, v_tab[eb])

    y = lax.map(block, (xt.reshape(nb, PEER_TOKEN_BLOCK, D),
                        experts.reshape(nb, PEER_TOKEN_BLOCK, PEER_HEADS, PEER_TOPK),
                        g.reshape(nb, PEER_TOKEN_BLOCK, PEER_HEADS, PEER_TOPK)))
    return y.reshape(B, S, D)


def setup_inputs(seed: int = 0) -> dict:
    key = jax.random.key(seed)
    ks = jax.random.split(key, 24)
    f32 = jnp.float32
    L = DEPTH

    def nrm(k, shape, scale):
        return jax.random.normal(k, shape, f32) * scale

    return {
        "x": jax.random.normal(ks[0], (BATCH, SEQ, D_MODEL), f32),
        "norm1_g": 1.0 + nrm(ks[1], (L, D_MODEL), 0.02),
        "w_in": nrm(ks[2], (L, D_MODEL, D_IN), D_MODEL ** -0.5),
        "lambda_q1": nrm(ks[3], (L, DA_DQK), 0.1),
        "lambda_k1": nrm(ks[4], (L, DA_DQK), 0.1),
        "lambda_q2": nrm(ks[5], (L, DA_DQK), 0.1),
        "lambda_k2": nrm(ks[6], (L, DA_DQK), 0.1),
        "da_subln_g": 1.0 + nrm(ks[7], (L, DA_DV), 0.02),
        "sg_ln_g": 1.0 + nrm(ks[8], (L, SG_WIDTH), 0.02),
        "sg_ln_b": nrm(ks[9], (L, SG_WIDTH), 0.02),
        "sg_w": nrm(ks[10], (L, SG_GROUPS, SG_BLOCK, SG_BLOCK), 0.5 * SG_BLOCK ** -0.5),
        "sg_b": 1.0 + nrm(ks[11], (L, SG_GROUPS, SG_BLOCK), 0.02),
        "w_branch_attn": nrm(ks[12], (L, DA_WIDTH, D_MODEL), DA_WIDTH ** -0.5),
        "w_branch_sg": nrm(ks[13], (L, SG_WIDTH, D_MODEL), SG_WIDTH ** -0.5),
        "w_out": nrm(ks[14], (L, D_MODEL, D_MODEL), D_MODEL ** -0.5),
        "norm2_g": 1.0 + nrm(ks[15], (L, D_MODEL), 0.02),
        "peer_w_query": nrm(ks[16], (L, D_MODEL, PEER_HEADS * D_KEY), D_MODEL ** -0.5),
        "peer_subkeys": nrm(ks[17], (L, 2, N_KEYS, D_KEY_HALF), D_KEY_HALF ** -0.5),
        "peer_u": nrm(ks[18], (L, N_EXPERTS, D_MODEL), D_MODEL ** -0.5),
        "peer_v": nrm(ks[19], (L, N_EXPERTS, D_MODEL), 0.3),
        "final_g": 1.0 + nrm(ks[20], (D_MODEL,), 0.02),
    }


def reference(x, norm1_g, w_in, lambda_q1, lambda_k1, lambda_q2, lambda_k2, da_subln_g,
              sg_ln_g, sg_ln_b, sg_w, sg_b, w_branch_attn, w_branch_sg, w_out,
              norm2_g, peer_w_query, peer_subkeys, peer_u, peer_v, final_g):
    B, S, D = x.shape
    offsets = []
    acc = 0
    for sz in SPLIT_SIZES[:-1]:
        acc += sz
        offsets.append(acc)
    h = x
    for l in range(DEPTH):
        lam_init = lambda_init_for(l)
        xn = rmsnorm(h, norm1_g[l])
        proj = xn @ w_in[l]
        q, k, v, su, sv, gates = jnp.split(proj, offsets, axis=-1)
        q = q.reshape(B, S, 2, DA_HEADS, DA_DQK)
        k = k.reshape(B, S, 2, DA_HEADS, DA_DQK)
        v = v.reshape(B, S, DA_HEADS, DA_DV)
        lam = (jnp.exp(jnp.sum(lambda_q1[l].astype(jnp.float32) * lambda_k1[l].astype(jnp.float32)))
               - jnp.exp(jnp.sum(lambda_q2[l].astype(jnp.float32) * lambda_k2[l].astype(jnp.float32)))
               + lam_init)
        ya = diff_attention(q, k, v, lam, da_subln_g[l], lam_init)
        yb = spatial_gating(jax.nn.gelu(su, approximate=False), jax.nn.gelu(sv, approximate=False),
                            sg_ln_g[l], sg_ln_b[l], sg_w[l], sg_b[l])
        ga, gb = jnp.split(jax.nn.sigmoid(gates), 2, axis=-1)
        merged = ga * (ya @ w_branch_attn[l]) + gb * (yb @ w_branch_sg[l])
        h = h + merged @ w_out[l]
        h = h + peer(rmsnorm(h, norm2_g[l]), peer_w_query[l], peer_subkeys[l], peer_u[l], peer_v[l])
    return rmsnorm(h, final_g)
```

```python
from contextlib import ExitStack
import numpy as np
import concourse.bass as bass
import concourse.mybir as mybir
from concourse.bass_utils import run_bass_kernel_spmd

F32 = mybir.dt.float32
BF16 = mybir.dt.bfloat16
U32 = mybir.dt.uint32
AF = mybir.ActivationFunctionType
ALU = mybir.AluOpType
AX = mybir.AxisListType

S_LEN = 4096
D = 1024
NT = S_LEN // 128
EPS = 1e-6
LAM_INIT = 0.8 - 0.6 * 1.0
NEG = -1e30


class Buf:
    __slots__ = ("name", "w", "r", "dsem", "dcnt")

    def __init__(self, name):
        self.name = name
        self.w = None
        self.r = {}
        self.dsem = None
        self.dcnt = 0


class Sched:
    ENGS = ("pe", "act", "dve", "pool", "sp")

    def __init__(self, nc):
        self.nc = nc
        self.streams = {k: [] for k in self.ENGS}
        self.csem = {k: nc.alloc_semaphore("cs_" + k) for k in self.ENGS}
        self.ndsem = 0
        self.fuse_waits = True

    def _dsem(self, buf):
        if buf.dsem is None:
            buf.dsem = self.nc.alloc_semaphore("ds_%d" % self.ndsem)
            self.ndsem += 1
        return buf.dsem

    def op(self, eng, fn, reads=(), writes=(), dma=None):
        deps = set()
        for b in reads:
            if b.w is not None:
                deps.add(b.w)
        for b in writes:
            if b.w is not None and not (b.w[0] == "c" and b.w[1] == eng and dma is None):
                deps.add(b.w)
            for t in b.r.values():
                if not (t[0] == "c" and t[1] == eng and dma is None):
                    deps.add(t)
        st = self.streams[eng]
        idx = len(st)
        if dma is not None:
            sem = self._dsem(dma)
            dma.dcnt += 16
            tok = ("d", sem, dma.dcnt)
            key = ("d", sem.num)
        else:
            tok = ("c", eng, idx)
            key = eng
        if eng == "pe":
            deps = {d for d in deps if not (d[0] == "c" and d[1] == "pe")}
        deps.discard(tok)
        st.append({"fn": fn, "deps": deps, "sig": False, "dma": dma is not None, "tok": tok})
        for b in reads:
            b.r[key] = tok
        for b in writes:
            b.w = tok
            b.r = {}
        return tok

    def finish_waits(self, eng, bufs):
        deps = set()
        for b in bufs:
            if b.w is not None:
                deps.add(b.w)
            deps.update(b.r.values())
        self.streams[eng].append({"fn": None, "deps": deps, "sig": False, "dma": False, "tok": None})

    def emit(self):
        for e in self.ENGS:
            for ins in self.streams[e]:
                for d in ins["deps"]:
                    if d[0] == "c":
                        self.streams[d[1]][d[2]]["sig"] = True
        signum = {}
        for e in self.ENGS:
            c = 0
            for i, ins in enumerate(self.streams[e]):
                if ins["sig"]:
                    c += 1
                    signum[(e, i)] = c
        self.nwait = 0
        bname = {"pe": "tensor", "act": "scalar", "dve": "vector", "pool": "gpsimd", "sp": "sync"}
        with self.nc.Block() as block:
            for e in self.ENGS:
                getattr(block, bname[e])(lambda eo, e=e: self._emit_stream(e, eo, signum))
        return self.nwait

    def _emit_stream(self, e, eo, signum):
        seen = {}
        for i, ins in enumerate(self.streams[e]):
            need = {}
            for d in ins["deps"]:
                if d[0] == "c":
                    sem, val = self.csem[d[1]], signum[(d[1], d[2])]
                else:
                    sem, val = d[1], d[2]
                k = sem.num
                if seen.get(k, 0) >= val:
                    continue
                if k not in need or need[k][1] < val:
                    need[k] = (sem, val)
            waits = list(need.values())
            for k, (sem, val) in need.items():
                seen[k] = val
            fuse = None
            if ins["fn"] is not None and waits and self.fuse_waits:
                fuse = waits.pop()
            for sem, val in waits:
                eo.wait_ge(sem, val)
                self.nwait += 1
            if ins["fn"] is None:
                continue
            r = ins["fn"](eo)
            if fuse is not None:
                r._wait_ge(fuse[0], fuse[1])
            if ins["dma"]:
                r.then_inc(ins["tok"][1], 16)
            elif ins["sig"]:
                r.then_inc(self.csem[e], 1)


def bc(ap, pos, n):
    dims = [list(d) for d in ap.ap]
    dims.insert(1 + pos, [0, n])
    return bass.AP(ap.tensor, ap.offset, dims)


def pbc(ap, n=128):
    dims = [list(d) for d in ap.ap]
    dims[0] = [0, n]
    return bass.AP(ap.tensor, ap.offset, dims)


class K:
    def __init__(self, nc, dbg=(), stop_after=None, lim=None):
        self.lim = lim or {}
        self.nc = nc
        self.S = Sched(nc)
        self.dbg = set(dbg)
        self.stop_after = stop_after
        self.dbg_out = {}
        self._n = 0

    def sb(self, name, shape, dt):
        self._n += 1
        return self.stk.enter_context(self.nc.sbuf_tensor("%s_%d" % (name, self._n), list(shape), dt))

    def B(self, name):
        b = Buf(name)
        self.phase_bufs.append(b)
        return b

    def fence(self):
        for e in Sched.ENGS:
            self.S.finish_waits(e, self.phase_bufs)
        self.phase_bufs = []

    def din(self, name, shape, dt=F32):
        return self.nc.dram_tensor(name, list(shape), dt, kind="ExternalInput").ap()

    def dscr(self, name, shape, dt):
        return self.nc.dram_tensor(name, list(shape), dt, kind="Internal").ap()

    def dout(self, name, shape, dt=F32):
        return self.nc.dram_tensor(name, list(shape), dt, kind="ExternalOutput").ap()

    def op(self, *a, **k):
        return self.S.op(*a, **k)

    def dump(self, name, src_ap, src_buf, shape, dt):
        if name not in self.dbg:
            return
        o = self.dout("dbg_" + name, shape, dt)
        self.dbg_out[name] = o
        self.op("sp", lambda e: e.dma_start(out=o, in_=src_ap), reads=[src_buf], writes=[self.b_out], dma=self.b_dbg)

    def build(self):
        self.phase_bufs = []
        with ExitStack() as top:
            self.stk = top
            return self._build(top)

    def _build(self, top):
        nc, S, op = self.nc, self.S, self.op
        sb = self.sb
        self.b_out = Buf("out")
        self.b_dbg = Buf("dbgsem")

        x = self.din("x", [S_LEN, D])
        WhR = self.din("WhR", [8, 128, 8 * 384])
        WgR = self.din("WgR", [8, 128, 8 * 512])
        WbrR = self.din("WbrR", [6, 128, 8 * 512])
        vecs = self.din("vecs", [8, 1024])
        sgwT = self.din("sgwT", [128, 8 * 128])
        sgb = self.din("sgb", [1, 8 * 128])
        ident_d = self.din("ident", [128, 128])
        wqR = self.din("wqR", [4, 128, 8 * 512])
        skT = self.din("skT", [128, 2 * 128])
        UTR = self.din("UTR", [128, 128, 1024])
        VR = self.din("VR", [128, 128, 1024])
        iota_d = self.din("iotas", [128, 2048 + 128])
        out = self.dout("out", [S_LEN, D])
        wqS = self.dscr("wqS", [4, 128, 8 * 512], BF16)
        UTS = self.dscr("UTS", [128, 128, 1024], BF16)
        VS = self.dscr("VS", [128, 128, 1024], BF16)
        self.p2 = dict(wqR=wqR, wqS=wqS, UTR=UTR, UTS=UTS, VR=VR, VS=VS)

        WhS = self.dscr("WhS", [8, 128, 8 * 384], BF16)
        WgS = self.dscr("WgS", [8, 128, 8 * 512], BF16)
        WbrS = self.dscr("WbrS", [6, 128, 8 * 512], BF16)
        yaT_d = self.dscr("yaT_d", [8, 128, S_LEN], BF16)
        hbuf = self.dscr("hbuf", [S_LEN, D], F32)

        b_WhS, b_WgS, b_WbrS = [Buf("WhS%d" % h) for h in range(8)], Buf("WgS"), Buf("WbrS")
        for h in range(8):
            op("pool", lambda e, h=h: e.dma_start(out=WhS[h], in_=WhR[h]), writes=[b_WhS[h]], dma=b_WhS[h])
        for g in range(8):
            op("pool", lambda e, g=g: e.dma_start(out=WgS[g], in_=WgR[g]), writes=[b_WgS], dma=b_WgS)
        for g in range(6):
            op("pool", lambda e, g=g: e.dma_start(out=WbrS[g], in_=WbrR[g]), writes=[b_WbrS], dma=b_WbrS)

        b_par = Buf("params")
        b_c = Buf("consts")
        self.b_c = b_c
        ident_f = sb("ident_f", [128, 128], F32)
        ident_b = sb("ident_b", [128, 128], BF16)
        self.ident_b, self.ident_f = ident_b, ident_f
        self._eps = sb("eps_t", [128, 1], F32)
        self.vecs, self.iota_d, self.skT_d = vecs, iota_d, skT
        sx = ExitStack()
        self.stk = sx
        lngb = sb("lngb", [128, 1024], F32)
        lnbb = sb("lnbb", [128, 1024], F32)
        sub_g = sb("sub_g", [128, 128], F32)
        lamv = sb("lamv", [128, 256], F32)
        bsb = sb("bsb", [128, 8 * 128], F32)
        wmT = sb("wmT", [128, 8, 128], BF16)
        for dst, src in ((ident_f[:], ident_d), (lngb[:], pbc(vecs[1:2, :])),
                         (lnbb[:], pbc(vecs[2:3, :])), (sub_g[:], pbc(vecs[5:6, 0:128])),
                         (lamv[:], pbc(vecs[6:7, 0:256])), (bsb[:], pbc(sgb[0:1, :]))):
            op("sp", lambda e, dst=dst, src=src: e.dma_start(out=dst, in_=src), writes=[b_par], dma=b_par)
        op("dve", lambda e: e.memset(self._eps[:], EPS), writes=[b_c])
        op("dve", lambda e: e.tensor_copy(out=ident_b[:], in_=ident_f[:]), reads=[b_par], writes=[b_c])
        op("dve", lambda e: e.tensor_scalar(out=sub_g[:], in0=sub_g[:], scalar1=float(1.0 - LAM_INIT), scalar2=None, op0=ALU.mult),
           reads=[b_par], writes=[b_c])
        lam_t = sb("lam_t", [128, 128], F32)
        lam_s = sb("lam_s", [128, 2], F32)
        lam_e = sb("lam_e", [128, 2], F32)
        nlam = sb("nlam", [128, 1], F32)
        lv = lamv[:].rearrange("p (a b) -> p a b", a=4)
        op("dve", lambda e: e.tensor_tensor(out=lam_t[:, 0:64], in0=lv[:, 0, :], in1=lv[:, 1, :], op=ALU.mult), reads=[b_par], writes=[b_c])
        op("dve", lambda e: e.tensor_tensor(out=lam_t[:, 64:128], in0=lv[:, 2, :], in1=lv[:, 3, :], op=ALU.mult), reads=[b_c], writes=[b_c])
        op("dve", lambda e: e.tensor_reduce(out=lam_s[:], in_=lam_t[:].rearrange("p (a b) -> p a b", a=2), axis=AX.X, op=ALU.add),
           reads=[b_c], writes=[b_c])
        op("act", lambda e: e.activation(out=lam_e[:], in_=lam_s[:], func=AF.Exp), reads=[b_c], writes=[b_c])
        op("dve", lambda e: e.scalar_tensor_tensor(out=nlam[:], in0=lam_e[:, 1:2], scalar=float(-LAM_INIT), in1=lam_e[:, 0:1],
                                                   op0=ALU.add, op1=ALU.subtract), reads=[b_c], writes=[b_c])

        ps = [nc.alloc_psum_tensor("ps%d" % i, [128, 512], F32) for i in range(8)]
        pb = [Buf("ps%d" % i) for i in range(8)]
        self.ps, self.pb = ps, pb

        self.x, self.out, self.hbuf = x, out, hbuf
        xnT = sb("xnT", [128, 8, S_LEN], BF16)
        b_xnT = [Buf("xnT%d" % i) for i in range(NT)]
        self.xnT, self.b_xnT = xnT, b_xnT
        with ExitStack() as s1a:
            self.stk = s1a
            self.alloc_rms()
            g1b = sb("g1b", [128, 1024], F32)
            wmT_f = sb("wmT_f", [128, 8 * 128], F32)
            b_par1 = self.B("par1")
            op("sp", lambda e: e.dma_start(out=g1b[:], in_=pbc(vecs[0:1, :])), writes=[b_par1], dma=b_par1)
            op("sp", lambda e: e.dma_start(out=wmT_f[:], in_=sgwT), writes=[b_par1], dma=b_par1)
            op("dve", lambda e: e.tensor_copy(out=wmT[:].rearrange("p g i -> p (g i)"), in_=wmT_f[:]), reads=[b_par1], writes=[b_c])
            op("dve", lambda e: e.memset(wmT[64:128, :, 0:64], 0.0), reads=[], writes=[b_c])
            xt, b_xt = self.xt, self.b_xt
            xnb, b_xnb = self.rms_tmp[2], self.rms_tmp[3]
            for tt in range(NT):
                i = tt % 2
                op("sp", lambda e, tt=tt, i=i: e.dma_start(out=xt[i][:], in_=x[tt * 128:(tt + 1) * 128, :]), writes=[b_xt[i]], dma=b_xt[i])
                self.rmsnorm_tile(xt[i], b_xt[i], g1b, i, tt, b_g=b_par1)
                self.transpose_tile(xnb[i], b_xnb[i], xnT[:, :, tt * 128:(tt + 1) * 128], [b_xnT[tt]], tt)
            if "xnT" in self.dbg:
                self.dump("xnT", xnT[:].rearrange("p c t -> p (c t)"), b_xnT[NT - 1], [128, 8 * S_LEN], BF16)
            self.fence()
        if self.stop_after == "1a":
            sx.close()
            return self.finish(out)

        self.convert_tables()
        with ExitStack() as s1b:
            self.stk = s1b
            self.phase_1b(WhS, b_WhS, yaT_d, sub_g, nlam)
            self.fence()
        if self.stop_after == "1b":
            sx.close()
            return self.finish(out)

        with ExitStack() as s1c:
            self.stk = s1c
            self.phase_1c(WgS, b_WgS, WbrS, b_WbrS, yaT_d, lngb, lnbb, bsb, wmT)
            self.fence()
        sx.close()
        if self.stop_after == "1c":
            return self.finish(out)

        with ExitStack() as s2:
            self.stk = s2
            self.phase_2()
            self.fence()
        return self.finish(out)

    def convert_tables(self):
        op, p2 = self.op, self.p2
        self.b_wqS, self.b_UTS, self.b_VS = Buf("wqS"), Buf("UTS"), Buf("VS")
        for g in range(4):
            op("pool", lambda e, g=g: e.dma_start(out=p2["wqS"][g], in_=p2["wqR"][g]), writes=[self.b_wqS], dma=self.b_wqS)

    def convert_tables_part(self, h):
        op, p2 = self.op, self.p2
        for i in range(h * 16, (h + 1) * 16, 4):
            op("pool", lambda e, i=i: e.dma_start(out=p2["UTS"][i:i + 4], in_=p2["UTR"][i:i + 4]), writes=[self.b_UTS], dma=self.b_UTS)
            op("pool", lambda e, i=i: e.dma_start(out=p2["VS"][i:i + 4], in_=p2["VR"][i:i + 4]), writes=[self.b_VS], dma=self.b_VS)

    def alloc_rms(self):
        sb, B = self.sb, self.B
        self.xt = [sb("xt%d" % i, [128, 1024], F32) for i in range(2)]
        self.b_xt = [B("xt%d" % i) for i in range(2)]
        junk = sb("junk", [128, 1024], BF16)
        xnb = [sb("xnb%d" % i, [128, 1024], BF16) for i in range(2)]
        st = [sb("st%d" % i, [128, 4], F32) for i in range(2)]
        self.rms_tmp = (junk, B("junk"), xnb, [B("xnb%d" % i) for i in range(2)], st, [B("st%d" % i) for i in range(2)])

    def transpose_tile(self, src, b_src, dst_ap, b_dst, n, bank=None):
        op, ps, pb = self.op, self.ps, self.pb
        if bank is None:
            bank = n % 2
        pv = ps[bank][:].bitcast(BF16).rearrange("p (c t) -> p c t", c=8)
        for c in range(8):
            op("pe", lambda e, c=c, pv=pv: e.transpose(out=pv[:, c, :], in_=src[:, c * 128:(c + 1) * 128], identity=self.ident_b[:]),
               reads=[b_src, self.b_c], writes=[pb[bank]])
        if n % 2 == 0:
            op("act", lambda e, pv=pv: e.activation(out=dst_ap, in_=pv, func=AF.Copy), reads=[pb[bank]], writes=b_dst)
        else:
            op("dve", lambda e, pv=pv: e.tensor_copy(out=dst_ap, in_=pv), reads=[pb[bank]], writes=b_dst)

    def phase_1b(self, WhS, b_WhS, yaT_d, sub_g, nlam):
        nc, op, sb = self.nc, self.op, self.sb
        ps, pb, xnT, b_xnT, b_c = self.ps, self.pb, self.xnT, self.b_xnT, self.b_c
        qT = sb("qT", [128, S_LEN], BF16)
        kT = sb("kT", [128, S_LEN], BF16)
        Vh = sb("Vh", [128, NT, 130], BF16)
        b_qT = [self.B("qT%d" % i) for i in range(8)]
        b_kT = [self.B("kT%d" % i) for i in range(8)]
        b_V = [self.B("V%d" % i) for i in range(8)]
        Wh = [sb("Wh%d" % i, [128, 8, 384], BF16) for i in range(2)]
        b_Wh = [self.B("Wh%d" % i) for i in range(2)]
        PT = [sb("PT%d" % i, [128, 512], BF16) for i in range(4)]
        b_PT = [self.B("PT%d" % i) for i in range(4)]
        ya = [sb("ya%d" % i, [128, 4, 128], BF16) for i in range(2)]
        b_ya = [self.B("ya%d" % i) for i in range(2)]
        yaTs = [sb("yaTs%d" % i, [128, 512], BF16) for i in range(2)]
        b_yaTs = [self.B("yaTs%d" % i) for i in range(2)]
        ev = [sb("ev%d" % i, [128, 8], F32) for i in range(2)]
        b_ev = [self.B("ev%d" % i) for i in range(2)]
        o1t = [sb("o1t%d" % i, [128, 128], F32) for i in range(2)]
        o2t = [sb("o2t%d" % i, [128, 128], F32) for i in range(2)]
        osq = [sb("osq%d" % i, [128, 128], F32) for i in range(2)]
        b_ot = [self.B("ot%d" % i) for i in range(2)]
        self.b_yaT_d = Buf("yaT_d")
        eps = self.eps_ap()

        op("pool", lambda e: e.memset(Vh[:, :, 128:130], 1.0), writes=b_V)

        cp = [0]

        def evac_copy(out_ap, in_ap, reads, writes):
            cp[0] += 1
            if cp[0] % 2 == 0:
                op("act", lambda e: e.activation(out=out_ap, in_=in_ap, func=AF.Copy), reads=reads, writes=writes)
            else:
                op("dve", lambda e: e.tensor_copy(out=out_ap, in_=in_ap), reads=reads, writes=writes)

        evn = [0]
        Osb = [sb("Osb%d" % i, [128, 4, 258], F32) for i in range(2)]
        b_Osb = [self.B("Osb%d" % i) for i in range(2)]
        pending = [None]

        def emit_transposes(yi, h, qb):
            tb = 0
            pv = ps[tb][:].bitcast(BF16)[:, 0:512].rearrange("p (j t) -> p j t", j=4)
            for j in range(4):
                op("pe", lambda e, yi=yi, j=j, pv=pv: e.transpose(out=pv[:, j, :], in_=ya[yi][:, j, :], identity=self.ident_b[:]),
                   reads=[b_ya[yi], b_c], writes=[pb[tb]])
            evac_copy(yaTs[yi][:], ps[tb][:].bitcast(BF16)[:, 0:512], [pb[tb]], [b_yaTs[yi]])
            op("sp", lambda e, yi=yi, h=h, qb=qb: e.dma_start(out=yaT_d[h, :, qb * 512:(qb + 1) * 512], in_=yaTs[yi][:]),
               reads=[b_yaTs[yi]], writes=[self.b_yaT_d], dma=b_yaTs[yi])

        for h in range(self.lim.get('heads', 8)):
            wi = h % 2
            W = Wh[wi]
            op("sp", lambda e, h=h, W=W: e.dma_start(out=W[:].rearrange("p k c -> p (k c)"), in_=WhS[h]),
               reads=[b_WhS[h]], writes=[b_Wh[wi]], dma=b_Wh[wi])
            for which, dst, bdst in ((0, qT, b_qT), (1, kT, b_kT)):
                for nb in range(8):
                    bank = nb % 4
                    for kc in range(8):
                        op("pe", lambda e, kc=kc, nb=nb, bank=bank, which=which, W=W: e.matmul(
                            ps[bank][:, :], lhsT=W[:, kc, which * 128:(which + 1) * 128], rhs=xnT[:, kc, nb * 512:(nb + 1) * 512],
                            start=(kc == 0), stop=(kc == 7)),
                           reads=[b_Wh[wi]] + b_xnT[nb * 4:(nb + 1) * 4], writes=[pb[bank]])
                    evac_copy(dst[:, nb * 512:(nb + 1) * 512], ps[bank][:, :], [pb[bank]], [bdst[nb]])
            for grp in range(8):
                bank = grp % 4
                for t in range(4):
                    tt = grp * 4 + t
                    for kc in range(8):
                        op("pe", lambda e, kc=kc, tt=tt, t=t, bank=bank, W=W: e.matmul(
                            ps[bank][:, t * 128:(t + 1) * 128], lhsT=xnT[:, kc, tt * 128:(tt + 1) * 128], rhs=W[:, kc, 256:384],
                            start=(kc == 0), stop=(kc == 7)),
                           reads=[b_Wh[wi], b_xnT[tt]], writes=[pb[bank]])
                evac_copy(Vh[:, grp * 4:(grp + 1) * 4, 0:128], ps[bank][:, :].rearrange("p (t c) -> p t c", t=4), [pb[bank]], [b_V[grp]])
            self.convert_tables_part(h)
            for qb in range(self.lim.get('qbs', 8)):
                nkt = 4 * qb + 4
                def emit_S(kt):
                    jmin = max(0, kt - 4 * qb)
                    nq = 512 - 128 * jmin
                    q0 = qb * 512 + jmin * 128
                    for m in range(2):
                        sbank = m * 2 + (kt % 2)
                        op("pe", lambda e, m=m, kt=kt, sbank=sbank, nq=nq, q0=q0: e.matmul(
                            ps[sbank][:, 0:nq], lhsT=kT[m * 64:(m + 1) * 64, kt * 128:(kt + 1) * 128],
                            rhs=qT[m * 64:(m + 1) * 64, q0:q0 + nq], start=True, stop=True),
                           reads=[b_kT[kt // 4], b_qT[qb]], writes=[pb[sbank]])

                emit_S(0)
                for kt in range(nkt):
                    jmin = max(0, kt - 4 * qb)
                    nq = 512 - 128 * jmin
                    if kt + 1 < nkt:
                        emit_S(kt + 1)
                    for m in range(2):
                        sbank = m * 2 + (kt % 2)
                        r = (kt * 2 + m) % 4
                        op("act", lambda e, sbank=sbank, r=r, nq=nq: e.activation(out=PT[r][:, 0:nq], in_=ps[sbank][:, 0:nq], func=AF.Exp, scale=0.125),
                           reads=[pb[sbank]], writes=[b_PT[r]])
                        if kt >= 4 * qb:
                            op("dve", lambda e, r=r: e.memset(PT[r][64:128, 0:64], 0.0), writes=[b_PT[r]])
                    for m in range(2):
                        r = (kt * 2 + m) % 4
                        for j in range(jmin, 4):
                            obank = 4 + m * 2 + j // 2
                            off = (j % 2) * 129
                            first = (kt == 0 and j % 2 == 0)
                            op("pe", lambda e, r=r, j=j, jmin=jmin, kt=kt, obank=obank, off=off, first=first: e.matmul(
                                ps[obank][:, off:off + 129], lhsT=PT[r][:, (j - jmin) * 128:(j - jmin + 1) * 128], rhs=Vh[:, kt, 0:129],
                                start=first, stop=(kt == 4 * qb + j), skip_group_check=True),
                               reads=[b_PT[r], b_V[kt // 4]], writes=[pb[obank]])
                if pending[0] is not None:
                    emit_transposes(*pending[0])
                    pending[0] = None
                yi = (h * 8 + qb) % 2
                Os, b_Os = Osb[yi], b_Osb[yi]
                for b4 in range(4):
                    op("dve", lambda e, b4=b4, Os=Os: e.tensor_copy(out=Os[:, b4, :], in_=ps[4 + b4][:, 0:258]), reads=[pb[4 + b4]], writes=[b_Os])
                for j in range(4):
                    ei = evn[0] % 2
                    evn[0] += 1
                    e_t, o1, o2, sq = ev[ei], o1t[ei], o2t[ei], osq[ei]
                    off = (j % 2) * 129
                    b1, b2 = 4 + j // 2, 6 + j // 2
                    O1 = Os[:, b1 - 4, off:off + 129]
                    O2 = Os[:, b2 - 4, off:off + 129]
                    op("dve", lambda e, e_t=e_t, O1=O1: e.reciprocal(out=e_t[:, 0:1], in_=O1[:, 128:129]), reads=[b_Os], writes=[b_ev[ei]])
                    op("dve", lambda e, e_t=e_t, O2=O2: e.reciprocal(out=e_t[:, 1:2], in_=O2[:, 128:129]), reads=[b_Os], writes=[b_ev[ei]])
                    op("dve", lambda e, e_t=e_t: e.tensor_tensor(out=e_t[:, 2:3], in0=e_t[:, 1:2], in1=nlam[:], op=ALU.mult),
                       reads=[b_ev[ei], b_c], writes=[b_ev[ei]])
                    op("dve", lambda e, e_t=e_t, O1=O1, o1=o1: e.tensor_scalar(out=o1[:], in0=O1[:, 0:128], scalar1=e_t[:, 0:1], scalar2=None, op0=ALU.mult),
                       reads=[b_Os, b_ev[ei]], writes=[b_ot[ei]])
                    op("dve", lambda e, e_t=e_t, O2=O2, o1=o1, o2=o2: e.scalar_tensor_tensor(
                        out=o2[:], in0=O2[:, 0:128], scalar=e_t[:, 2:3], in1=o1[:], op0=ALU.mult, op1=ALU.add),
                       reads=[b_Os, b_ev[ei], b_ot[ei]], writes=[b_ot[ei]])
                    op("dve", lambda e, e_t=e_t, o2=o2, sq=sq: e.scalar_tensor_tensor(
                        out=sq[:], in0=o2[:], scalar=1.0, in1=o2[:], op0=ALU.mult, op1=ALU.mult, accum_out=e_t[:, 3:4]),
                       reads=[b_ot[ei]], writes=[b_ot[ei], b_ev[ei]])
                    op("act", lambda e, e_t=e_t: e.activation(out=e_t[:, 4:5], in_=e_t[:, 3:4], func=AF.Ln, scale=1.0 / 128, bias=eps),
                       reads=[b_ev[ei], b_c], writes=[b_ev[ei]])
                    op("act", lambda e, e_t=e_t: e.activation(out=e_t[:, 5:6], in_=e_t[:, 4:5], func=AF.Exp, scale=-0.5),
                       reads=[b_ev[ei]], writes=[b_ev[ei]])
                    op("dve", lambda e, e_t=e_t, o2=o2, yi=yi, j=j: e.scalar_tensor_tensor(
                        out=ya[yi][:, j, :], in0=o2[:], scalar=e_t[:, 5:6], in1=sub_g[:], op0=ALU.mult, op1=ALU.mult),
                       reads=[b_ot[ei], b_ev[ei], b_c], writes=[b_ya[yi]])
                pending[0] = (yi, h, qb)
        if pending[0] is not None:
            emit_transposes(*pending[0])
        if "yaT" in self.dbg:
            o = self.dout("dbg_yaT", [8, 128, S_LEN], BF16)
            for h in range(8):
                op("pool", lambda e, h=h: e.dma_start(out=o[h], in_=yaT_d[h]), reads=[self.b_yaT_d], writes=[self.b_out], dma=self.b_dbg)

    def phase_1c(self, WgS, b_WgS, WbrS, b_WbrS, yaT_d, lngb, lnbb, bsb, wmT):
        nc, op, sb, B = self.nc, self.op, self.sb, self.B
        ps, pb, xnT, b_xnT, b_c = self.ps, self.pb, self.xnT, self.b_xnT, self.b_c
        x, hbuf = self.x, self.hbuf
        NR = 4
        Wr = [sb("Wr%d" % i, [128, 8, 512], BF16) for i in range(NR)]
        b_Wr = [B("Wr%d" % i) for i in range(NR)]
        uT = sb("uT", [128, 8, 512], BF16); b_uT = B("uT")
        gT = sb("gT", [128, 16, 512], BF16); b_gT = B("gT")
        gvf = [sb("gvf%d" % i, [128, 1024], F32) for i in range(2)]
        b_gvf = [B("gvf%d" % i) for i in range(2)]
        lst = [sb("lst%d" % i, [128, 8], F32) for i in range(2)]
        b_lst = [B("lst%d" % i) for i in range(2)]
        vln = [sb("vln%d" % i, [128, 1024], BF16) for i in range(4)]
        b_vln = [B("vln%d" % i) for i in range(4)]
        yaTb = sb("yaTb", [128, 8, 512], BF16); b_yaTb = B("yaTb")
        ybT = sb("ybT", [128, 8, 512], BF16); b_ybT = B("ybT")
        mT = sb("mT", [128, 8, 512], BF16); b_mT = B("mT")
        t1 = [sb("t1_%d" % i, [128, 512], F32) for i in range(2)]
        t2 = [sb("t2_%d" % i, [128, 512], F32) for i in range(2)]
        b_t1 = [B("t1_%d" % i) for i in range(2)]
        b_t2 = [B("t2_%d" % i) for i in range(2)]
        xt = [sb("xt%d" % i, [128, 1024], F32) for i in range(2)]
        b_xt = [B("xt%d" % i) for i in range(2)]
        self.b_hbuf = Buf("hbuf")
        eps = self.eps_ap()

        nblk = self.lim.get("blocks", 8)
        seq = []
        for nb in range(nblk):
            seq += [(WgS, b_WgS, g) for g in range(8)]
            seq += [(WbrS, b_WbrS, g) for g in (0, 2, 1, 3, 4, 5)]
        issued = [0]

        def wslot(i, ahead=NR):
            while issued[0] < min(len(seq), i + ahead):
                k = issued[0]
                src, bsrc, g = seq[k]
                sl = k % NR
                op("sp", lambda e, src=src, g=g, sl=sl: e.dma_start(out=Wr[sl][:].rearrange("p k c -> p (k c)"), in_=src[g]),
                   reads=[bsrc], writes=[b_Wr[sl]], dma=b_Wr[sl])
                issued[0] += 1
            return i % NR

        bk = [0]

        def nbank():
            bk[0] = (bk[0] + 1) % 8
            return bk[0]

        wi = 0
        for nb in range(nblk):
            tok = slice(nb * 512, (nb + 1) * 512)
            bx = b_xnT[nb * 4:(nb + 1) * 4]
            op("sp", lambda e, nb=nb: e.dma_start(out=yaTb[:], in_=yaT_d[:, :, nb * 512:(nb + 1) * 512].rearrange("h p t -> p h t")),
               reads=[self.b_yaT_d], writes=[b_yaTb], dma=b_yaTb)
            for gi in range(2):
                sl = wslot(wi); wi += 1
                for c in range(4):
                    bank = nbank()
                    for kc in range(8):
                        op("pe", lambda e, kc=kc, c=c, sl=sl, bank=bank, tok=tok: e.matmul(
                            ps[bank][:, :], lhsT=Wr[sl][:, kc, c * 128:(c + 1) * 128], rhs=xnT[:, kc, tok], start=(kc == 0), stop=(kc == 7)),
                           reads=[b_Wr[sl]] + bx, writes=[pb[bank]])
                    op("act", lambda e, bank=bank, ch=gi * 4 + c: e.activation(out=uT[:, ch, :], in_=ps[bank][:, :], func=AF.Gelu),
                       reads=[pb[bank]], writes=[b_uT])
            sl0 = wslot(wi); sl1 = wslot(wi + 1, NR - 1); wi += 2
            for t in range(4):
                tt = nb * 4 + t
                gi_ = t % 2
                G, st_ = gvf[gi_], lst[gi_]
                for half, sl in ((0, sl0), (1, sl1)):
                    bank = nbank()
                    for kc in range(8):
                        op("pe", lambda e, kc=kc, sl=sl, bank=bank, tt=tt: e.matmul(
                            ps[bank][:, :], lhsT=xnT[:, kc, tt * 128:(tt + 1) * 128], rhs=Wr[sl][:, kc, :], start=(kc == 0), stop=(kc == 7)),
                           reads=[b_Wr[sl], b_xnT[tt]], writes=[pb[bank]])
                    op("act", lambda e, bank=bank, G=G, st_=st_, half=half: e.activation(
                        out=G[:, half * 512:(half + 1) * 512], in_=ps[bank][:, :], func=AF.Gelu, accum_out=st_[:, half:half + 1]),
                       reads=[pb[bank]], writes=[b_gvf[gi_], b_lst[gi_]])
                op("dve", lambda e, G=G, st_=st_, t=t: e.scalar_tensor_tensor(out=vln[t][:], in0=G[:], scalar=1.0, in1=G[:], op0=ALU.mult, op1=ALU.mult,
                                                                      accum_out=st_[:, 2:3]), reads=[b_gvf[gi_]], writes=[b_vln[t], b_lst[gi_]])
                op("dve", lambda e, st_=st_: e.tensor_tensor(out=st_[:, 3:4], in0=st_[:, 0:1], in1=st_[:, 1:2], op=ALU.add), reads=[b_lst[gi_]], writes=[b_lst[gi_]])
                op("dve", lambda e, st_=st_: e.tensor_scalar(out=st_[:, 3:4], in0=st_[:, 3:4], scalar1=1.0 / 1024, scalar2=None, op0=ALU.mult),
                   reads=[b_lst[gi_]], writes=[b_lst[gi_]])
                op("dve", lambda e, st_=st_: e.tensor_tensor(out=st_[:, 4:5], in0=st_[:, 3:4], in1=st_[:, 3:4], op=ALU.mult), reads=[b_lst[gi_]], writes=[b_lst[gi_]])
                op("dve", lambda e, st_=st_: e.scalar_tensor_tensor(out=st_[:, 5:6], in0=st_[:, 2:3], scalar=1.0 / 1024, in1=st_[:, 4:5],
                                                                 op0=ALU.mult, op1=ALU.subtract), reads=[b_lst[gi_]], writes=[b_lst[gi_]])
                op("act", lambda e, st_=st_: e.activation(out=st_[:, 6:7], in_=st_[:, 5:6], func=AF.Sqrt, bias=eps), reads=[b_lst[gi_], b_c], writes=[b_lst[gi_]])
                op("dve", lambda e, st_=st_: e.reciprocal(out=st_[:, 7:8], in_=st_[:, 6:7]), reads=[b_lst[gi_]], writes=[b_lst[gi_]])
                op("dve", lambda e, G=G, st_=st_: e.tensor_scalar(out=G[:], in0=G[:], scalar1=st_[:, 3:4], scalar2=st_[:, 7:8], op0=ALU.subtract, op1=ALU.mult),
                   reads=[b_gvf[gi_], b_lst[gi_]], writes=[b_gvf[gi_]])
                op("dve", lambda e, G=G: e.tensor_tensor(out=G[:], in0=G[:], in1=lngb[:], op=ALU.mult), reads=[b_gvf[gi_], b_c], writes=[b_gvf[gi_]])
                op("dve", lambda e, G=G, t=t: e.tensor_tensor(out=vln[t][:], in0=G[:], in1=lnbb[:], op=ALU.add), reads=[b_gvf[gi_], b_c], writes=[b_vln[t]])
            for gi in range(4):
                sl = wslot(wi); wi += 1
                for c in range(4):
                    bank = nbank()
                    for kc in range(8):
                        op("pe", lambda e, kc=kc, c=c, sl=sl, bank=bank, tok=tok: e.matmul(
                            ps[bank][:, :], lhsT=Wr[sl][:, kc, c * 128:(c + 1) * 128], rhs=xnT[:, kc, tok], start=(kc == 0), stop=(kc == 7)),
                           reads=[b_Wr[sl]] + bx, writes=[pb[bank]])
                    op("act", lambda e, bank=bank, ch=gi * 4 + c: e.activation(out=gT[:, ch, :], in_=ps[bank][:, :], func=AF.Sigmoid),
                       reads=[pb[bank]], writes=[b_gT])
            for g in range(8):
                bank = nbank()
                for t in range(4):
                    op("pe", lambda e, g=g, t=t, bank=bank: e.matmul(
                        ps[bank][:, t * 128:(t + 1) * 128], lhsT=vln[t][:, g * 128:(g + 1) * 128], rhs=wmT[:, g, :], start=True, stop=True),
                       reads=[b_vln[t], b_c], writes=[pb[bank]])
                i2 = g % 2
                op("dve", lambda e, g=g, bank=bank, i2=i2: e.tensor_tensor(
                    out=t1[i2][:].rearrange("p (t i) -> p t i", t=4), in0=ps[bank][:, :].rearrange("p (t i) -> p t i", t=4),
                    in1=bc(bsb[:, g * 128:(g + 1) * 128], 0, 4), op=ALU.add), reads=[pb[bank], b_c], writes=[b_t1[i2]])
                op("dve", lambda e, g=g, i2=i2: e.tensor_tensor(out=ybT[:, g, :], in0=t1[i2][:], in1=uT[:, g, :], op=ALU.mult),
                   reads=[b_t1[i2], b_uT], writes=[b_ybT])
            for gi in range(2):
                slA = wslot(wi); slB = wslot(wi + 1, NR - 1); wi += 2
                for c in range(4):
                    fo = gi * 4 + c
                    i2 = fo % 2
                    bankA = nbank()
                    for kc in range(8):
                        op("pe", lambda e, kc=kc, c=c, slA=slA, bankA=bankA: e.matmul(
                            ps[bankA][:, :], lhsT=Wr[slA][:, kc, c * 128:(c + 1) * 128], rhs=yaTb[:, kc, :], start=(kc == 0), stop=(kc == 7)),
                           reads=[b_Wr[slA], b_yaTb], writes=[pb[bankA]])
                    bankB = nbank()
                    for kc in range(8):
                        op("pe", lambda e, kc=kc, c=c, slB=slB, bankB=bankB: e.matmul(
                            ps[bankB][:, :], lhsT=Wr[slB][:, kc, c * 128:(c + 1) * 128], rhs=ybT[:, kc, :], start=(kc == 0), stop=(kc == 7)),
                           reads=[b_Wr[slB], b_ybT], writes=[pb[bankB]])
                    op("dve", lambda e, bankA=bankA, fo=fo, i2=i2: e.tensor_tensor(out=t1[i2][:], in0=ps[bankA][:, :], in1=gT[:, fo, :], op=ALU.mult),
                       reads=[pb[bankA], b_gT], writes=[b_t1[i2]])
                    op("dve", lambda e, bankB=bankB, fo=fo, i2=i2: e.tensor_tensor(out=t2[i2][:], in0=ps[bankB][:, :], in1=gT[:, 8 + fo, :], op=ALU.mult),
                       reads=[pb[bankB], b_gT], writes=[b_t2[i2]])
                    op("dve", lambda e, fo=fo, i2=i2: e.tensor_tensor(out=mT[:, fo, :], in0=t1[i2][:], in1=t2[i2][:], op=ALU.add),
                       reads=[b_t1[i2], b_t2[i2]], writes=[b_mT])
            slo0 = wslot(wi); slo1 = wslot(wi + 1, NR - 1); wi += 2
            for t in range(4):
                tt = nb * 4 + t
                i2 = t % 2
                op("sp", lambda e, tt=tt, i2=i2: e.dma_start(out=xt[i2][:], in_=x[tt * 128:(tt + 1) * 128, :]), writes=[b_xt[i2]], dma=b_xt[i2])
                for half, sl in ((0, slo0), (1, slo1)):
                    bank = nbank()
                    for kc in range(8):
                        op("pe", lambda e, kc=kc, sl=sl, bank=bank, t=t: e.matmul(
                            ps[bank][:, :], lhsT=mT[:, kc, t * 128:(t + 1) * 128], rhs=Wr[sl][:, kc, :], start=(kc == 0), stop=(kc == 7)),
                           reads=[b_Wr[sl], b_mT], writes=[pb[bank]])
                    op("dve", lambda e, bank=bank, half=half, i2=i2: e.tensor_tensor(
                        out=xt[i2][:, half * 512:(half + 1) * 512], in0=ps[bank][:, :], in1=xt[i2][:, half * 512:(half + 1) * 512], op=ALU.add),
                       reads=[pb[bank], b_xt[i2]], writes=[b_xt[i2]])
                op("sp", lambda e, tt=tt, i2=i2: e.dma_start(out=hbuf[tt * 128:(tt + 1) * 128, :], in_=xt[i2][:]),
                   reads=[b_xt[i2]], writes=[self.b_hbuf], dma=b_xt[i2])
        if "h" in self.dbg:
            o = self.dout("dbg_h", [S_LEN, D], F32)
            for q in range(8):
                op("pool", lambda e, q=q: e.dma_start(out=o[q * 512:(q + 1) * 512, :], in_=hbuf[q * 512:(q + 1) * 512, :]),
                   reads=[self.b_hbuf], writes=[self.b_out], dma=self.b_dbg)

    def phase_2(self):
        nc, op, sb, B = self.nc, self.op, self.sb, self.B
        ps, pb, b_c = self.ps, self.pb, self.b_c
        p2 = self.p2
        hbuf, out = self.hbuf, self.out
        vecs, iota_d = self.vecs, self.iota_d
        n2gb = sb("n2gb", [128, 1024], F32)
        fgb = sb("fgb", [128, 1024], F32)
        iota16 = sb("iota16", [128, 2048], F32)
        iota128f = sb("iota128f", [128, 128], F32)
        iota128 = sb("iota128", [128, 128], BF16)
        skT_f = sb("skT_f", [128, 256], F32)
        skTb = sb("skTb", [128, 2, 128], BF16)
        b_p2 = B("par2")
        for dst, src in ((n2gb[:], pbc(vecs[3:4, :])), (fgb[:], pbc(vecs[4:5, :])), (iota16[:], iota_d[:, 0:2048]),
                         (iota128f[:], iota_d[:, 2048:2176]), (skT_f[:], self.skT_d)):
            op("sp", lambda e, dst=dst, src=src: e.dma_start(out=dst, in_=src), writes=[b_p2], dma=b_p2)
        op("dve", lambda e: e.tensor_copy(out=iota128[:], in_=iota128f[:]), reads=[b_p2], writes=[b_p2])
        op("dve", lambda e: e.tensor_copy(out=skTb[:].rearrange("p a n -> p (a n)"), in_=skT_f[:]), reads=[b_p2], writes=[b_p2])
        b_c2 = b_p2
        self.rcst = sb("rcst", [128, 2], F32)
        self.b_rcst = B("rcst")
        op("dve", lambda e: e.memset(self.rcst[:, 0:1], float(D * EPS)), writes=[self.b_rcst])
        op("dve", lambda e: e.memset(self.rcst[:, 1:2], -0.5), writes=[self.b_rcst])
        self.ecst = sb("ecst", [128, 128], F32)
        op("dve", lambda e: e.memset(self.ecst[:], float(np.e)), writes=[self.b_rcst])
        TP = 256
        nblk = self.lim.get("pblocks", S_LEN // TP)
        NCH = self.lim.get("chunks", 128)
        self.alloc_rms()
        xnb, b_xnb = self.rms_tmp[2], self.rms_tmp[3]
        xtp = [self.xt, [sb("xtB%d" % i, [128, 1024], F32) for i in range(2)]]
        b_xtp = [self.b_xt, [B("xtB%d" % i) for i in range(2)]]
        xn2Tp = [sb("xn2T%d" % i, [128, 8, TP], BF16) for i in range(3)]
        b_xn2Tp = [B("xn2T%d" % i) for i in range(3)]
        pqT = sb("pqT", [128, 16, TP], BF16); b_pqT = B("pqT")
        Wq = [sb("Wq%d" % i, [128, 8, 256], BF16) for i in range(2)]
        b_Wq = [B("Wq%d" % i) for i in range(2)]
        sc = sb("sc", [128, 16, 128], F32); b_sc = [B("sc%d" % i) for i in range(16)]
        svt = sb("svt", [128, 16, 16], F32)
        sit = sb("sit", [128, 16, 16], U32); b_sv = [B("sv%d" % i) for i in range(16)]
        cand = sb("cand", [128, 8, 256], F32); b_cand = [B("cand%d" % i) for i in range(8)]
        ts = sb("ts", [128, 8, 16], F32)
        pos = sb("pos", [128, 8, 16], U32); b_ts = [B("ts%d" % i) for i in range(8)]
        ee = sb("ee", [128, 8, 16], F32)
        zz = sb("zz", [128, 16], F32)
        gg = sb("gg", [128, 8, 16], BF16); b_g = B("g")
        pa = sb("pa", [128, 128], U32)
        pbb = sb("pbb", [128, 128], U32)
        af = sb("af", [128, 8, 16], F32)
        bf_ = sb("bf_", [128, 8, 16], F32)
        sif = sb("sif", [128, 16, 16], F32); b_ix = B("ix")
        II = sb("II", [128, 128], BF16)
        JJ = sb("JJ", [128, 128], BF16); b_IJ = B("IJ")
        IJGp = [sb("IJG%d" % i, [128, 3, TP], BF16) for i in range(2)]
        b_IJGp = [B("IJG%d" % i) for i in range(2)]
        SBK = 16
        A = [sb("A%d" % i, [128, SBK, 128], BF16) for i in range(2)]
        Bm = [sb("Bm%d" % i, [128, SBK, 128], BF16) for i in range(2)]
        b_A = [B("A%d" % i) for i in range(2)]
        b_Bm = [B("Bm%d" % i) for i in range(2)]
        CL = 60
        WH = 128 - CL
        WsbL = sb("WsbL", [128, TP, CL], BF16); b_WsbL = B("WsbL")
        WsbH = sb("WsbH", [128, TP, WH], BF16); b_WsbH = B("WsbH")
        stg = [sb("stg%d" % i, [128, SBK, WH], BF16) for i in range(2)]
        b_stg = [B("stg%d" % i) for i in range(2)]
        WH_d = self.dscr("WH_d", [2, 128, TP * WH], BF16)
        b_WHd = [B("WHd%d" % i) for i in range(2)]
        NRING = 6
        UT = [sb("UT%d" % i, [128, 8, 128], BF16) for i in range(NRING)]
        Vt = [sb("Vt%d" % i, [128, 1024], BF16) for i in range(NRING)]
        b_UT = [B("UT%d" % i) for i in range(NRING)]
        b_Vt = [B("Vt%d" % i) for i in range(NRING)]
        Gt = [sb("Gt%d" % i, [128, TP], BF16) for i in range(3)]
        Mt = [sb("Mt%d" % i, [128, TP], BF16) for i in range(3)]
        b_Gt = [B("Gt%d" % i) for i in range(3)]
        b_Mt = [B("Mt%d" % i) for i in range(3)]

        bk = [0]

        def nbank():
            bk[0] = (bk[0] + 1) % 4
            return bk[0]

        cp = [0]

        def evac_copy(out_ap, in_ap, reads, writes, act_share=3):
            cp[0] += 1
            if cp[0] % 3 < act_share:
                op("act", lambda e: e.activation(out=out_ap, in_=in_ap, func=AF.Copy), reads=reads, writes=writes)
            else:
                op("dve", lambda e: e.tensor_copy(out=out_ap, in_=in_ap), reads=reads, writes=writes)

        def top16(srcs, vals, idxs, b_srcs, b_dsts, every=8, pad=0):
            n = len(srcs)

            def brk(k):
                if k % every == every - 1:
                    for _ in range(1 + pad):
                        yield

            for r in range(2):
                sl = slice(r * 8, (r + 1) * 8)
                for k in range(n):
                    op("dve", lambda e, k=k, sl=sl: e.max(out=vals[k][:, sl], in_=srcs[k]), reads=[b_srcs[k]], writes=[b_dsts[k]])
                    yield from brk(k)
                for k in range(n):
                    op("dve", lambda e, k=k, sl=sl: e.max_index(out=idxs[k][:, sl], in_max=vals[k][:, sl], in_values=srcs[k]),
                       reads=[b_srcs[k], b_dsts[k]], writes=[b_dsts[k]])
                    yield from brk(k)
                if r == 0:
                    for k in range(n):
                        op("dve", lambda e, k=k, sl=sl: e.match_replace(out=srcs[k], in_to_replace=vals[k][:, sl], in_values=srcs[k], imm_value=NEG),
                           reads=[b_srcs[k], b_dsts[k]], writes=[b_srcs[k]])
                        yield from brk(k)

        total = nblk * NCH
        issued = [0]

        def tload(upto, oldest):
            while issued[0] < min(total, upto + 1, oldest + NRING):
                k = issued[0]
                i = k % NCH
                sl = k % NRING
                op("sp", lambda e, i=i, sl=sl: e.dma_start(out=UT[sl][:].rearrange("p k j -> p (k j)"), in_=p2["UTS"][i]),
                   reads=[self.b_UTS], writes=[b_UT[sl]], dma=b_UT[sl])
                op("sp", lambda e, i=i, sl=sl: e.dma_start(out=Vt[sl][:], in_=p2["VS"][i]),
                   reads=[self.b_VS], writes=[b_Vt[sl]], dma=b_Vt[sl])
                issued[0] += 1

        def pro_AB(blk):
            xt, b_xt, xn2T, b_xn2T = xtp[0], b_xtp[0], xn2Tp[blk % 3], b_xn2Tp[blk % 3]
            wq4 = p2["wqS"].rearrange("g p (k c) -> g p k c", k=8)

            def wq_load(hg):
                op("sp", lambda e, hg=hg: e.dma_start(out=Wq[hg % 2][:], in_=wq4[hg // 2, :, :, (hg % 2) * 256:(hg % 2 + 1) * 256]),
                   reads=[self.b_wqS], writes=[b_Wq[hg % 2]], dma=b_Wq[hg % 2])

            for t in range(2):
                tt = blk * 2 + t
                op("sp", lambda e, tt=tt, t=t: e.dma_start(out=xt[t][:], in_=hbuf[tt * 128:(tt + 1) * 128, :]),
                   reads=[self.b_hbuf], writes=[b_xt[t]], dma=b_xt[t])
            wq_load(0)
            for _ in range(4):
                yield
            for t in range(2):
                self.rms_sq(xt[t], b_xt[t], t)
            yield
            for t in range(2):
                tt = blk * 2 + t
                self.rms_fin(xt[t], b_xt[t], n2gb, t, b_g=b_p2)
                yield
                self.transpose_tile(xnb[t], b_xnb[t], xn2T[:, :, t * 128:(t + 1) * 128], [b_xn2T], tt, bank=3)
            yield
            for hg in range(8):
                sl = hg % 2
                if hg + 1 < 8:
                    wq_load(hg + 1)
                for c in range(2):
                    bank = 2
                    for kc in range(8):
                        op("pe", lambda e, kc=kc, c=c, sl=sl, bank=bank: e.matmul(
                            ps[bank][:, 0:TP], lhsT=Wq[sl][:, kc, c * 128:(c + 1) * 128], rhs=xn2T[:, kc, :], start=(kc == 0), stop=(kc == 7)),
                           reads=[b_Wq[sl], b_xn2T], writes=[pb[bank]])
                    evac_copy(pqT[:, hg * 2 + c, :], ps[bank][:, 0:TP], [pb[bank]], [b_pqT])
                    yield
            yield

        def pro_chain(blk):
            par = blk % 2
            IJG, b_IJG = IJGp[par], b_IJGp[par]
            for t in range(2):
                for bq in range(4):
                    bank = 3
                    for k in range(4):
                        hp = bq * 4 + k
                        op("pe", lambda e, hp=hp, k=k, t=t, bank=bank: e.matmul(
                            ps[bank][:, k * 128:(k + 1) * 128], lhsT=pqT[:, hp, t * 128:(t + 1) * 128], rhs=skTb[:, hp % 2, :], start=True, stop=True),
                           reads=[b_pqT, b_c2], writes=[pb[bank]])
                    evac_copy(sc[:, bq * 4:(bq + 1) * 4, :], ps[bank][:, :].rearrange("p (a n) -> p a n", a=4), [pb[bank]], b_sc[bq * 4:(bq + 1) * 4])
                    yield
                yield
                yield from top16([sc[:, hp, :] for hp in range(16)], [svt[:, hp, :] for hp in range(16)], [sit[:, hp, :] for hp in range(16)], b_sc, b_sv,
                                 every=8, pad=1)
                svv = svt[:].rearrange("p (h two) k -> p h two k", two=2)
                op("dve", lambda e, svv=svv: e.tensor_tensor(out=cand[:].rearrange("p h (a b) -> p h a b", a=16),
                                                             in0=bc(svv[:, :, 0, :], 2, 16), in1=bc(svv[:, :, 1, :], 1, 16), op=ALU.add),
                   reads=b_sv, writes=b_cand)
                yield
                yield
                yield from top16([cand[:, h, :] for h in range(8)], [ts[:, h, :] for h in range(8)], [pos[:, h, :] for h in range(8)], b_cand, b_ts,
                                 every=4, pad=0)
                op("dve", lambda e: e.tensor_tensor(out=ee[:], in0=ts[:], in1=bc(ts[:, :, 0], 1, 16), op=ALU.subtract), reads=b_ts, writes=[b_g])
                yield
                yield
                op("pool", lambda e: e.tensor_tensor(out=ee[:].rearrange("p h k -> p (h k)"), in0=self.ecst[:], in1=ee[:].rearrange("p h k -> p (h k)"), op=ALU.pow),
                   reads=[b_g, self.b_rcst], writes=[b_g])
                yield
                op("dve", lambda e: e.tensor_reduce(out=zz[:, 0:8], in_=ee[:], axis=AX.X, op=ALU.add), reads=[b_g], writes=[b_g])
                op("dve", lambda e: e.reciprocal(out=zz[:, 8:16], in_=zz[:, 0:8]), reads=[b_g], writes=[b_g])
                op("dve", lambda e: e.tensor_tensor(out=gg[:], in0=ee[:], in1=bc(zz[:, 8:16], 1, 16), op=ALU.mult), reads=[b_g], writes=[b_g])
                pos2 = pos[:].rearrange("p h k -> p (h k)")
                op("dve", lambda e, pos2=pos2: e.tensor_single_scalar(out=pa[:], in_=pos2, scalar=4, op=ALU.logical_shift_right), reads=b_ts, writes=[b_ix])
                op("dve", lambda e, pos2=pos2: e.tensor_single_scalar(out=pbb[:], in_=pos2, scalar=15, op=ALU.bitwise_and), reads=b_ts, writes=[b_ix])
                op("dve", lambda e: e.tensor_copy(out=af[:].rearrange("p h k -> p (h k)"), in_=pa[:]), reads=[b_ix], writes=[b_ix])
                op("dve", lambda e: e.tensor_copy(out=bf_[:].rearrange("p h k -> p (h k)"), in_=pbb[:]), reads=[b_ix], writes=[b_ix])
                op("dve", lambda e: e.tensor_copy(out=sif[:], in_=sit[:]), reads=b_sv, writes=[b_ix])
                yield
                sifv = sif[:].rearrange("p (h two) k -> p h two k", two=2)
                eq4 = cand[:].rearrange("p h (a b) -> p h a b", a=16)
                io4 = iota16[:].rearrange("p (h a b) -> p h a b", h=8, a=16)
                for which, sel, dst in ((0, af, II), (1, bf_, JJ)):
                    op("dve", lambda e, sel=sel: e.tensor_tensor(out=eq4, in0=bc(sel[:], 2, 16), in1=io4, op=ALU.is_equal),
                       reads=[b_ix, b_c2] + b_ts, writes=b_cand)
                    yield
                    op("dve", lambda e, which=which: e.tensor_tensor(out=eq4, in0=eq4, in1=bc(sifv[:, :, which, :], 1, 16), op=ALU.mult),
                       reads=b_cand + [b_ix], writes=b_cand)
                    yield
                    def red(e, dst=dst):
                        with self.nc.allow_low_precision("sum of one-hot * small integer index: exact in bf16"):
                            return e.tensor_reduce(out=dst[:].rearrange("p (h k) -> p h k", h=8), in_=eq4, axis=AX.X, op=ALU.add)
                    op("dve", red, reads=b_cand, writes=[b_IJ])
                    yield
                yield
                bank = 3
                for n_, src in enumerate((II[:], JJ[:], gg[:].rearrange("p h k -> p (h k)"))):
                    rd = [b_IJ, b_c] if n_ < 2 else [b_g, b_c]
                    op("pe", lambda e, n_=n_, src=src, bank=bank: e.transpose(out=ps[bank][:].bitcast(BF16)[:, n_ * 128:(n_ + 1) * 128], in_=src, identity=self.ident_b[:]),
                       reads=rd, writes=[pb[bank]])
                evac_copy(IJG[:, :, t * 128:(t + 1) * 128], ps[bank][:].bitcast(BF16)[:, 0:384].rearrange("p (a n) -> p a n", a=3), [pb[bank]], [b_IJG])
                yield

        jkc = [0]

        def stage_JK(blk, banks):
            par = blk % 2
            IJG, b_IJG = IJGp[par], b_IJGp[par]
            WHv = WH_d[par].rearrange("p (t i) -> p t i", i=WH)
            io3 = bc(iota128[:], 0, SBK)

            def emit_dve(sbk, ai):
                t0 = sbk * SBK
                for tk in range(SBK):
                    tok = t0 + tk
                    op("dve", lambda e, ai=ai, tk=tk, tok=tok: e.tensor_scalar(
                        out=A[ai][:, tk, :], in0=iota128[:], scalar1=IJG[:, 0, tok:tok + 1], scalar2=IJG[:, 2, tok:tok + 1],
                        op0=ALU.is_equal, op1=ALU.mult), reads=[b_IJG, b_c2], writes=[b_A[ai]])
                op("dve", lambda e, ai=ai, t0=t0: e.tensor_tensor(out=Bm[ai][:], in0=io3, in1=bc(IJG[:, 1, t0:t0 + SBK], 1, 128), op=ALU.is_equal),
                   reads=[b_IJG, b_c2], writes=[b_Bm[ai]])

            def emit_group(sbk, ai, q4):
                jkc[0] += 1
                bank = banks[jkc[0] % len(banks)]
                for k in range(4):
                    tk = q4 * 4 + k
                    op("pe", lambda e, ai=ai, tk=tk, k=k, bank=bank: e.matmul(
                        ps[bank][:, k * 128:(k + 1) * 128], lhsT=Bm[ai][:, tk, :], rhs=A[ai][:, tk, :], start=True, stop=True),
                       reads=[b_A[ai], b_Bm[ai]], writes=[pb[bank]])
                pv = ps[bank][:, :].rearrange("p (t i) -> p t i", t=4)
                tok0 = sbk * SBK + q4 * 4
                r = sbk % 2
                op("act", lambda e, pv=pv, tok0=tok0: e.activation(out=WsbL[:, tok0:tok0 + 4, :], in_=pv[:, :, 0:CL], func=AF.Copy),
                   reads=[pb[bank]], writes=[b_WsbL])
                op("act", lambda e, pv=pv, r=r, q4=q4: e.activation(out=stg[r][:, q4 * 4:q4 * 4 + 4, :], in_=pv[:, :, CL:128], func=AF.Copy),
                   reads=[pb[bank]], writes=[b_stg[r]])

            def emit_spill(sbk):
                r = sbk % 2
                t0 = sbk * SBK
                op("sp", lambda e, r=r, t0=t0: e.dma_start(out=WHv[:, t0:t0 + SBK, :], in_=stg[r][:]),
                   reads=[b_stg[r]], writes=[b_WHd[par]], dma=b_WHd[par])

            nsb = TP // SBK
            emit_dve(0, 0)
            yield
            for sbk in range(nsb):
                for q4 in range(SBK // 4):
                    if q4 == 0 and sbk + 1 < nsb:
                        emit_dve(sbk + 1, (sbk + 1) % 2)
                    if q4 == 1 and sbk > 0:
                        emit_spill(sbk - 1)
                    emit_group(sbk, sbk % 2, q4)
                    yield
            emit_spill(nsb - 1)
            yield

        def fill(blk):
            par = blk % 2
            WHv = WH_d[par].rearrange("p (t i) -> p t i", i=WH)
            for sbk in range(TP // SBK):
                t0 = sbk * SBK
                op("sp", lambda e, t0=t0: e.dma_start(out=WsbH[:, t0:t0 + SBK, :], in_=WHv[:, t0:t0 + SBK, :]),
                   reads=[b_WHd[par]], writes=[b_WsbH], dma=b_WsbH)
                yield

        def step(g, n=1):
            if g is not None:
                for _ in range(n):
                    next(g, None)

        def flush(g):
            if g is not None:
                for _ in g:
                    pass

        def main_loop(blk, gF, gC, gL, gAB, gE):
            xn2T, b_xn2T = xn2Tp[blk % 3], b_xn2Tp[blk % 3]
            def emit_H(i):
                n = blk * NCH + i
                tload(n + 3, blk * NCH + max(0, i - 2))
                sl = n % NRING
                hb = n % 2
                for kc in range(8):
                    op("pe", lambda e, kc=kc, sl=sl, hb=hb: e.matmul(
                        ps[hb][:, 0:TP], lhsT=UT[sl][:, kc, :], rhs=xn2T[:, kc, :], start=(kc == 0), stop=(kc == 7)),
                       reads=[b_UT[sl], b_xn2T], writes=[pb[hb]])

            def emit_GM(i):
                n = blk * NCH + i
                hb = n % 2
                r3 = n % 3
                op("act", lambda e, hb=hb, r3=r3: e.activation(out=Gt[r3][:], in_=ps[hb][:, 0:TP], func=AF.Gelu), reads=[pb[hb]], writes=[b_Gt[r3]])
                Wv, b_Wv = (WsbL[:, :, i], b_WsbL) if i < CL else (WsbH[:, :, i - CL], b_WsbH)
                op("pool", lambda e, r3=r3, Wv=Wv: e.tensor_tensor(out=Mt[r3][:], in0=Gt[r3][:], in1=Wv, op=ALU.mult),
                   reads=[b_Gt[r3], b_Wv], writes=[b_Mt[r3]])

            emit_H(0)
            if NCH > 1:
                emit_H(1)
            emit_GM(0)
            for i in range(NCH):
                n = blk * NCH + i
                sl = n % NRING
                r3 = n % 3
                if i == 2:
                    flush(gE)
                if i == CL - 1:
                    flush(gF)
                    flush(gC)
                if i == NCH - 16:
                    for t in range(2):
                        tt = blk * 2 + t
                        op("sp", lambda e, tt=tt, t=t: e.dma_start(out=xtp[1][t][:], in_=hbuf[tt * 128:(tt + 1) * 128, :]),
                           reads=[self.b_hbuf], writes=[b_xtp[1][t]], dma=b_xtp[1][t])
                if i + 2 < NCH:
                    emit_H(i + 2)
                if i + 1 < NCH:
                    emit_GM(i + 1)
                if i < CL - 1:
                    step(gF)
                    if i < 2:
                        step(gE)
                    step(gC, 2)
                else:
                    step(gL)
                    if (i - CL) % 2 == 1:
                        step(gAB)
                for t in range(2):
                    for half in range(2):
                        yb_ = 4 + t * 2 + half
                        op("pe", lambda e, t=t, half=half, yb_=yb_, r3=r3, sl=sl, i=i: e.matmul(
                            ps[yb_][:, :], lhsT=Mt[r3][:, t * 128:(t + 1) * 128], rhs=Vt[sl][:, half * 512:(half + 1) * 512],
                            start=(i == 0), stop=(i == NCH - 1)),
                           reads=[b_Mt[r3], b_Vt[sl]], writes=[pb[yb_]])
            flush(gE)
            flush(gF)
            flush(gC)
            flush(gL)
            flush(gAB)

        def epilogue(blk):
            xt, b_xt = xtp[1], b_xtp[1]
            for t in range(2):
                for half in range(2):
                    yb_ = 4 + t * 2 + half
                    op("dve", lambda e, t=t, half=half, yb_=yb_: e.tensor_tensor(
                        out=xt[t][:, half * 512:(half + 1) * 512], in0=ps[yb_][:, :], in1=xt[t][:, half * 512:(half + 1) * 512], op=ALU.add),
                       reads=[pb[yb_], b_xt[t]], writes=[b_xt[t]])
            yield
            for t in range(2):
                self.rms_sq(xt[t], b_xt[t], t)
            yield
            for t in range(2):
                tt = blk * 2 + t
                self.rms_fin(xt[t], b_xt[t], fgb, t, out_ap=xt[t][:], b_o=b_xt[t], b_g=b_p2)
                op("sp", lambda e, tt=tt, t=t: e.dma_start(out=out[tt * 128:(tt + 1) * 128, :], in_=xt[t][:]),
                   reads=[b_xt[t]], writes=[self.b_out], dma=b_xt[t])
            yield

        flush(pro_AB(0))
        flush(pro_chain(0))
        flush(stage_JK(0, (0, 1, 2, 3)))
        if nblk > 1:
            flush(pro_AB(1))
        gE = None
        for blk in range(nblk):
            main_loop(blk, fill(blk),
                      pro_chain(blk + 1) if blk + 1 < nblk else None,
                      stage_JK(blk + 1, (3,)) if blk + 1 < nblk else None,
                      pro_AB(blk + 2) if blk + 2 < nblk else None,
                      gE)
            gE = epilogue(blk)
            next(gE)
        flush(gE)

    def rmsnorm_tile(self, xt, b_xt, gb, i, tag, out_ap=None, b_o=None, b_g=None):
        op = self.op
        junk, b_junk, xnb, b_xnb, st, b_st = self.rms_tmp
        s = st[i]
        if out_ap is None:
            out_ap, b_o = xnb[i][:], b_xnb[i]
        op("act", lambda e: e.activation(out=junk[:], in_=xt[:], func=AF.Square, accum_out=s[:, 0:1]),
           reads=[b_xt], writes=[b_junk, b_st[i]])
        op("act", lambda e: e.activation(out=s[:, 1:2], in_=s[:, 0:1], func=AF.Sqrt, scale=1.0 / D, bias=self.eps_ap()),
           reads=[b_st[i], self.b_c], writes=[b_st[i]])
        op("dve", lambda e: e.reciprocal(out=s[:, 2:3], in_=s[:, 1:2]), reads=[b_st[i]], writes=[b_st[i]])
        op("dve", lambda e: e.scalar_tensor_tensor(out=out_ap, in0=xt[:], scalar=s[:, 2:3], in1=gb[:], op0=ALU.mult, op1=ALU.mult),
           reads=[b_xt, b_st[i], self.b_c] + ([b_g] if b_g is not None else []), writes=[b_o])

    def rms_sq(self, xt, b_xt, i):
        junk, b_junk, xnb, b_xnb, st, b_st = self.rms_tmp
        s = st[i]
        self.op("act", lambda e: e.activation(out=junk[:], in_=xt[:], func=AF.Square, accum_out=s[:, 0:1]),
                reads=[b_xt], writes=[b_junk, b_st[i]])

    def rms_fin(self, xt, b_xt, gb, i, out_ap=None, b_o=None, b_g=None):
        op = self.op
        junk, b_junk, xnb, b_xnb, st, b_st = self.rms_tmp
        s = st[i]
        if out_ap is None:
            out_ap, b_o = xnb[i][:], b_xnb[i]
        cst = self.rcst
        op("pool", lambda e: e.tensor_tensor(out=s[:, 1:2], in0=s[:, 0:1], in1=cst[:, 0:1], op=ALU.add),
           reads=[b_st[i], self.b_rcst], writes=[b_st[i]])
        op("pool", lambda e: e.tensor_tensor(out=s[:, 3:4], in0=s[:, 1:2], in1=cst[:, 1:2], op=ALU.pow),
           reads=[b_st[i], self.b_rcst], writes=[b_st[i]])
        op("dve", lambda e: e.tensor_scalar(out=s[:, 2:3], in0=s[:, 3:4], scalar1=float(D ** 0.5), scalar2=None, op0=ALU.mult),
           reads=[b_st[i]], writes=[b_st[i]])
        op("dve", lambda e: e.scalar_tensor_tensor(out=out_ap, in0=xt[:], scalar=s[:, 2:3], in1=gb[:], op0=ALU.mult, op1=ALU.mult),
           reads=[b_xt, b_st[i], self.b_c] + ([b_g] if b_g is not None else []), writes=[b_o])

    def eps_ap(self):
        return self._eps[:]

    def finish(self, out):
        self.S.finish_waits("sp", [self.b_out, self.b_dbg])
        return self.S.emit()


def _arr_kc(w, ncols_group):
    C = w.shape[1]
    G = C // ncols_group
    a = w.reshape(8, 128, G, ncols_group).transpose(2, 1, 0, 3)
    return np.ascontiguousarray(a).reshape(G, 128, 8 * ncols_group)


def prep_shared(inp):
    f = np.float32
    w_in = np.asarray(inp["w_in"], f)[0]
    cols = []
    for h in range(8):
        c = []
        for blk in (0, 1024):
            for m in range(2):
                c.extend(range(blk + m * 512 + h * 64, blk + m * 512 + h * 64 + 64))
        c.extend(range(2048 + h * 128, 2048 + h * 128 + 128))
        cols.append(c)
    WhR = np.concatenate([_arr_kc(w_in[:, c], 384) for c in cols], axis=0)
    WgR = _arr_kc(w_in[:, 3072:], 512)
    WbrR = np.concatenate([_arr_kc(np.asarray(inp[k], f)[0], 512) for k in ("w_branch_attn", "w_branch_sg", "w_out")], axis=0)
    vecs = np.zeros((8, 1024), f)
    vecs[0] = np.asarray(inp["norm1_g"], f)[0]
    vecs[1] = np.asarray(inp["sg_ln_g"], f)[0]
    vecs[2] = np.asarray(inp["sg_ln_b"], f)[0]
    vecs[3] = np.asarray(inp["norm2_g"], f)[0]
    vecs[4] = np.asarray(inp["final_g"], f)
    vecs[5, :128] = np.asarray(inp["da_subln_g"], f)[0]
    vecs[6, 0:64] = np.asarray(inp["lambda_q1"], f)[0]
    vecs[6, 64:128] = np.asarray(inp["lambda_k1"], f)[0]
    vecs[6, 128:192] = np.asarray(inp["lambda_q2"], f)[0]
    vecs[6, 192:256] = np.asarray(inp["lambda_k2"], f)[0]
    sgw = np.asarray(inp["sg_w"], f)[0]
    sgwT = np.ascontiguousarray(sgw.transpose(2, 0, 1)).reshape(128, 8 * 128)
    sgb = np.asarray(inp["sg_b"], f)[0].reshape(1, 8 * 128)
    wqR = _arr_kc(np.asarray(inp["peer_w_query"], f)[0], 512)
    sk = np.asarray(inp["peer_subkeys"], f)[0]
    skT = np.ascontiguousarray(sk.transpose(2, 0, 1)).reshape(128, 256)
    U = np.asarray(inp["peer_u"], f)[0]
    UTR = np.ascontiguousarray(U.reshape(128, 128, 8, 128).transpose(0, 3, 2, 1)).reshape(128, 128, 1024)
    VR = np.asarray(inp["peer_v"], f)[0].reshape(128, 128, 1024)
    iotas = np.zeros((128, 2048 + 128), f)
    iotas[:, :2048] = (np.arange(2048) % 16)[None, :]
    iotas[:, 2048:] = np.arange(128)[None, :]
    return {"WhR": WhR, "WgR": WgR, "WbrR": WbrR, "vecs": vecs, "sgwT": sgwT, "sgb": sgb,
            "ident": np.eye(128, dtype=f), "wqR": wqR, "skT": skT, "UTR": UTR, "VR": VR, "iotas": iotas}


_CACHE = {}


def kernel(**inputs):
    x = np.asarray(inputs["x"], np.float32)
    shared = prep_shared(inputs)
    if "nc" not in _CACHE:
        nc = bass.Bass("TRN2", target_bir_lowering=False)
        K(nc).build()
        _CACHE["nc"] = nc
    nc = _CACHE["nc"]
    in_maps = [dict(shared, x=np.ascontiguousarray(x[b])) for b in range(8)]
    res = run_bass_kernel_spmd(nc, in_maps, core_ids=list(range(8)))
    return np.stack([np.asarray(r["out"], np.float32) for r in res.results], axis=0)
```

```python
from contextlib import ExitStack
import numpy as np
import concourse.bass as bass
import concourse.mybir as mybir
from concourse.bass_utils import run_bass_kernel_spmd

F32 = mybir.dt.float32
BF16 = mybir.dt.bfloat16
U32 = mybir.dt.uint32
AF = mybir.ActivationFunctionType
ALU = mybir.AluOpType
AX = mybir.AxisListType

S_LEN = 4096
D = 1024
NT = S_LEN // 128
EPS = 1e-6
LAM_INIT = 0.8 - 0.6 * 1.0
NEG = -1e30


class Buf:
    __slots__ = ("name", "w", "r", "dsem", "dcnt")

    def __init__(self, name):
        self.name = name
        self.w = None
        self.r = {}
        self.dsem = None
        self.dcnt = 0


class Sched:
    ENGS = ("pe", "act", "dve", "pool", "sp")

    def __init__(self, nc):
        self.nc = nc
        self.streams = {k: [] for k in self.ENGS}
        self.csem = {k: nc.alloc_semaphore("cs_" + k) for k in self.ENGS}
        self.ndsem = 0
        self.fuse_waits = True

    def _dsem(self, buf):
        if buf.dsem is None:
            buf.dsem = self.nc.alloc_semaphore("ds_%d" % self.ndsem)
            self.ndsem += 1
        return buf.dsem

    def op(self, eng, fn, reads=(), writes=(), dma=None):
        deps = set()
        for b in reads:
            if b.w is not None:
                deps.add(b.w)
        for b in writes:
            if b.w is not None and not (b.w[0] == "c" and b.w[1] == eng and dma is None):
                deps.add(b.w)
            for t in b.r.values():
                if not (t[0] == "c" and t[1] == eng and dma is None):
                    deps.add(t)
        st = self.streams[eng]
        idx = len(st)
        if dma is not None:
            sem = self._dsem(dma)
            dma.dcnt += 16
            tok = ("d", sem, dma.dcnt)
            key = ("d", sem.num)
        else:
            tok = ("c", eng, idx)
            key = eng
        if eng == "pe":
            deps = {d for d in deps if not (d[0] == "c" and d[1] == "pe")}
        deps.discard(tok)
        st.append({"fn": fn, "deps": deps, "sig": False, "dma": dma is not None, "tok": tok})
        for b in reads:
            b.r[key] = tok
        for b in writes:
            b.w = tok
            b.r = {}
        return tok

    def finish_waits(self, eng, bufs):
        deps = set()
        for b in bufs:
            if b.w is not None:
                deps.add(b.w)
            deps.update(b.r.values())
        self.streams[eng].append({"fn": None, "deps": deps, "sig": False, "dma": False, "tok": None})

    def emit(self):
        for e in self.ENGS:
            for ins in self.streams[e]:
                for d in ins["deps"]:
                    if d[0] == "c":
                        self.streams[d[1]][d[2]]["sig"] = True
        signum = {}
        for e in self.ENGS:
            c = 0
            for i, ins in enumerate(self.streams[e]):
                if ins["sig"]:
                    c += 1
                    signum[(e, i)] = c
        self.nwait = 0
        bname = {"pe": "tensor", "act": "scalar", "dve": "vector", "pool": "gpsimd", "sp": "sync"}
        with self.nc.Block() as block:
            for e in self.ENGS:
                getattr(block, bname[e])(lambda eo, e=e: self._emit_stream(e, eo, signum))
        return self.nwait

    def _emit_stream(self, e, eo, signum):
        seen = {}
        for i, ins in enumerate(self.streams[e]):
            need = {}
            for d in ins["deps"]:
                if d[0] == "c":
                    sem, val = self.csem[d[1]], signum[(d[1], d[2])]
                else:
                    sem, val = d[1], d[2]
                k = sem.num
                if seen.get(k, 0) >= val:
                    continue
                if k not in need or need[k][1] < val:
                    need[k] = (sem, val)
            waits = list(need.values())
            for k, (sem, val) in need.items():
                seen[k] = val
            fuse = None
            if ins["fn"] is not None and waits and self.fuse_waits:
                fuse = waits.pop()
            for sem, val in waits:
                eo.wait_ge(sem, val)
                self.nwait += 1
            if ins["fn"] is None:
                continue
            r = ins["fn"](eo)
            if fuse is not None:
                r._wait_ge(fuse[0], fuse[1])
            if ins["dma"]:
                r.then_inc(ins["tok"][1], 16)
            elif ins["sig"]:
                r.then_inc(self.csem[e], 1)


def bc(ap, pos, n):
    dims = [list(d) for d in ap.ap]
    dims.insert(1 + pos, [0, n])
    return bass.AP(ap.tensor, ap.offset, dims)


def pbc(ap, n=128):
    dims = [list(d) for d in ap.ap]
    dims[0] = [0, n]
    return bass.AP(ap.tensor, ap.offset, dims)


class K:
    def __init__(self, nc, dbg=(), stop_after=None, lim=None):
        self.lim = lim or {}
        self.nc = nc
        self.S = Sched(nc)
        self.dbg = set(dbg)
        self.stop_after = stop_after
        self.dbg_out = {}
        self._n = 0

    def sb(self, name, shape, dt):
        self._n += 1
        return self.stk.enter_context(self.nc.sbuf_tensor("%s_%d" % (name, self._n), list(shape), dt))

    def B(self, name):
        b = Buf(name)
        self.phase_bufs.append(b)
        return b

    def fence(self):
        for e in Sched.ENGS:
            self.S.finish_waits(e, self.phase_bufs)
        self.phase_bufs = []

    def din(self, name, shape, dt=F32):
        return self.nc.dram_tensor(name, list(shape), dt, kind="ExternalInput").ap()

    def dscr(self, name, shape, dt):
        return self.nc.dram_tensor(name, list(shape), dt, kind="Internal").ap()

    def dout(self, name, shape, dt=F32):
        return self.nc.dram_tensor(name, list(shape), dt, kind="ExternalOutput").ap()

    def op(self, *a, **k):
        return self.S.op(*a, **k)

    def dump(self, name, src_ap, src_buf, shape, dt):
        if name not in self.dbg:
            return
        o = self.dout("dbg_" + name, shape, dt)
        self.dbg_out[name] = o
        self.op("sp", lambda e: e.dma_start(out=o, in_=src_ap), reads=[src_buf], writes=[self.b_out], dma=self.b_dbg)

    def build(self):
        self.phase_bufs = []
        with ExitStack() as top:
            self.stk = top
            return self._build(top)

    def _build(self, top):
        nc, S, op = self.nc, self.S, self.op
        sb = self.sb
        self.b_out = Buf("out")
        self.b_dbg = Buf("dbgsem")

        x = self.din("x", [S_LEN, D])
        WhR = self.din("WhR", [8, 128, 8 * 384])
        WgR = self.din("WgR", [8, 128, 8 * 512])
        WbrR = self.din("WbrR", [6, 128, 8 * 512])
        vecs = self.din("vecs", [8, 1024])
        sgwT = self.din("sgwT", [128, 8 * 128])
        sgb = self.din("sgb", [1, 8 * 128])
        ident_d = self.din("ident", [128, 128])
        wqR = self.din("wqR", [4, 128, 8 * 512])
        skT = self.din("skT", [128, 2 * 128])
        UTR = self.din("UTR", [128, 128, 1024])
        VR = self.din("VR", [128, 128, 1024])
        iota_d = self.din("iotas", [128, 2048 + 128])
        out = self.dout("out", [S_LEN, D])
        wqS = self.dscr("wqS", [4, 128, 8 * 512], BF16)
        UTS = self.dscr("UTS", [128, 128, 1024], BF16)
        VS = self.dscr("VS", [128, 128, 1024], BF16)
        self.p2 = dict(wqR=wqR, wqS=wqS, UTR=UTR, UTS=UTS, VR=VR, VS=VS)

        WhS = self.dscr("WhS", [8, 128, 8 * 384], BF16)
        WgS = self.dscr("WgS", [8, 128, 8 * 512], BF16)
        WbrS = self.dscr("WbrS", [6, 128, 8 * 512], BF16)
        yaT_d = self.dscr("yaT_d", [8, 128, S_LEN], BF16)
        hbuf = self.dscr("hbuf", [S_LEN, D], F32)

        b_WhS, b_WgS, b_WbrS = [Buf("WhS%d" % h) for h in range(8)], Buf("WgS"), Buf("WbrS")
        op("pool", lambda e: e.dma_start(out=WhS[0], in_=WhR[0]), writes=[b_WhS[0]], dma=b_WhS[0])

        def late_casts():
            last = [self.b_xnT[NT - 1]]
            for h in range(1, 8):
                op("pool", lambda e, h=h: e.dma_start(out=WhS[h], in_=WhR[h]), reads=last, writes=[b_WhS[h]], dma=b_WhS[h])
            for g in range(8):
                op("pool", lambda e, g=g: e.dma_start(out=WgS[g], in_=WgR[g]), reads=last, writes=[b_WgS], dma=b_WgS)
            for g in range(6):
                op("pool", lambda e, g=g: e.dma_start(out=WbrS[g], in_=WbrR[g]), reads=last, writes=[b_WbrS], dma=b_WbrS)

        b_par = Buf("params")
        b_c = Buf("consts")
        self.b_c = b_c
        ident_f = sb("ident_f", [128, 128], F32)
        ident_b = sb("ident_b", [128, 128], BF16)
        self.ident_b, self.ident_f = ident_b, ident_f
        self._eps = sb("eps_t", [128, 1], F32)
        self.vecs, self.iota_d, self.skT_d = vecs, iota_d, skT
        sx = ExitStack()
        self.stk = sx
        lngb = sb("lngb", [128, 1024], F32)
        lnbb = sb("lnbb", [128, 1024], F32)
        sub_g = sb("sub_g", [128, 128], F32)
        lamv = sb("lamv", [128, 256], F32)
        bsb = sb("bsb", [128, 8 * 128], F32)
        wmT = sb("wmT", [128, 8, 128], BF16)
        for dst, src in ((ident_f[:], ident_d), (lngb[:], pbc(vecs[1:2, :])),
                         (lnbb[:], pbc(vecs[2:3, :])), (sub_g[:], pbc(vecs[5:6, 0:128])),
                         (lamv[:], pbc(vecs[6:7, 0:256])), (bsb[:], pbc(sgb[0:1, :]))):
            op("sp", lambda e, dst=dst, src=src: e.dma_start(out=dst, in_=src), writes=[b_par], dma=b_par)
        op("dve", lambda e: e.memset(self._eps[:], EPS), writes=[b_c])
        op("dve", lambda e: e.tensor_copy(out=ident_b[:], in_=ident_f[:]), reads=[b_par], writes=[b_c])
        op("dve", lambda e: e.tensor_scalar(out=sub_g[:], in0=sub_g[:], scalar1=float(1.0 - LAM_INIT), scalar2=None, op0=ALU.mult),
           reads=[b_par], writes=[b_c])
        lam_t = sb("lam_t", [128, 128], F32)
        lam_s = sb("lam_s", [128, 2], F32)
        lam_e = sb("lam_e", [128, 2], F32)
        nlam = sb("nlam", [128, 1], F32)
        lv = lamv[:].rearrange("p (a b) -> p a b", a=4)
        op("dve", lambda e: e.tensor_tensor(out=lam_t[:, 0:64], in0=lv[:, 0, :], in1=lv[:, 1, :], op=ALU.mult), reads=[b_par], writes=[b_c])
        op("dve", lambda e: e.tensor_tensor(out=lam_t[:, 64:128], in0=lv[:, 2, :], in1=lv[:, 3, :], op=ALU.mult), reads=[b_c], writes=[b_c])
        op("dve", lambda e: e.tensor_reduce(out=lam_s[:], in_=lam_t[:].rearrange("p (a b) -> p a b", a=2), axis=AX.X, op=ALU.add),
           reads=[b_c], writes=[b_c])
        op("act", lambda e: e.activation(out=lam_e[:], in_=lam_s[:], func=AF.Exp), reads=[b_c], writes=[b_c])
        op("dve", lambda e: e.scalar_tensor_tensor(out=nlam[:], in0=lam_e[:, 1:2], scalar=float(-LAM_INIT), in1=lam_e[:, 0:1],
                                                   op0=ALU.add, op1=ALU.subtract), reads=[b_c], writes=[b_c])

        ps = [nc.alloc_psum_tensor("ps%d" % i, [128, 512], F32) for i in range(8)]
        pb = [Buf("ps%d" % i) for i in range(8)]
        self.ps, self.pb = ps, pb

        self.x, self.out, self.hbuf = x, out, hbuf
        xnT = sb("xnT", [128, 8, S_LEN], BF16)
        b_xnT = [Buf("xnT%d" % i) for i in range(NT)]
        self.xnT, self.b_xnT = xnT, b_xnT
        with ExitStack() as s1a:
            self.stk = s1a
            self.alloc_rms()
            g1b = sb("g1b", [128, 1024], F32)
            wmT_f = sb("wmT_f", [128, 8 * 128], F32)
            b_par1 = self.B("par1")
            op("sp", lambda e: e.dma_start(out=g1b[:], in_=pbc(vecs[0:1, :])), writes=[b_par1], dma=b_par1)
            op("sp", lambda e: e.dma_start(out=wmT_f[:], in_=sgwT), writes=[b_par1], dma=b_par1)
            op("dve", lambda e: e.tensor_copy(out=wmT[:].rearrange("p g i -> p (g i)"), in_=wmT_f[:]), reads=[b_par1], writes=[b_c])
            op("dve", lambda e: e.memset(wmT[64:128, :, 0:64], 0.0), reads=[], writes=[b_c])
            xt = [sb("xt4_%d" % i, [128, 1024], F32) for i in range(4)]
            b_xt = [self.B("xt4_%d" % i) for i in range(4)]
            xnb, b_xnb = self.rms_tmp[2], self.rms_tmp[3]
            for tt in range(NT):
                i = tt % 2
                j = tt % 4
                op("sp", lambda e, tt=tt, j=j: e.dma_start(out=xt[j][:], in_=x[tt * 128:(tt + 1) * 128, :]), writes=[b_xt[j]], dma=b_xt[j])
                self.rmsnorm_tile(xt[j], b_xt[j], g1b, i, tt, b_g=b_par1)
                self.transpose_tile(xnb[i], b_xnb[i], xnT[:, :, tt * 128:(tt + 1) * 128], [b_xnT[tt]], tt)
            if "xnT" in self.dbg:
                self.dump("xnT", xnT[:].rearrange("p c t -> p (c t)"), b_xnT[NT - 1], [128, 8 * S_LEN], BF16)
            self.fence()
        if self.stop_after == "1a":
            sx.close()
            return self.finish(out)

        late_casts()
        self.convert_tables()
        with ExitStack() as s1b:
            self.stk = s1b
            self.phase_1b(WhS, b_WhS, yaT_d, sub_g, nlam)
            self.fence()
        if self.stop_after == "1b":
            sx.close()
            return self.finish(out)

        with ExitStack() as s1c:
            self.stk = s1c
            self.phase_1c(WgS, b_WgS, WbrS, b_WbrS, yaT_d, lngb, lnbb, bsb, wmT)
            self.fence()
        sx.close()
        if self.stop_after == "1c":
            return self.finish(out)

        with ExitStack() as s2:
            self.stk = s2
            self.phase_2()
            self.fence()
        return self.finish(out)

    def convert_tables(self):
        op, p2 = self.op, self.p2
        self.b_wqS, self.b_UTS, self.b_VS = Buf("wqS"), Buf("UTS"), Buf("VS")
        for g in range(4):
            op("pool", lambda e, g=g: e.dma_start(out=p2["wqS"][g], in_=p2["wqR"][g]), writes=[self.b_wqS], dma=self.b_wqS)

    def convert_tables_part(self, h):
        op, p2 = self.op, self.p2
        for i in range(h * 16, (h + 1) * 16, 4):
            op("pool", lambda e, i=i: e.dma_start(out=p2["UTS"][i:i + 4], in_=p2["UTR"][i:i + 4]), writes=[self.b_UTS], dma=self.b_UTS)
            op("pool", lambda e, i=i: e.dma_start(out=p2["VS"][i:i + 4], in_=p2["VR"][i:i + 4]), writes=[self.b_VS], dma=self.b_VS)

    def alloc_rms(self):
        sb, B = self.sb, self.B
        self.xt = [sb("xt%d" % i, [128, 1024], F32) for i in range(2)]
        self.b_xt = [B("xt%d" % i) for i in range(2)]
        junk = sb("junk", [128, 1024], BF16)
        xnb = [sb("xnb%d" % i, [128, 1024], BF16) for i in range(2)]
        st = [sb("st%d" % i, [128, 4], F32) for i in range(2)]
        self.rms_tmp = (junk, B("junk"), xnb, [B("xnb%d" % i) for i in range(2)], st, [B("st%d" % i) for i in range(2)])

    def transpose_tile(self, src, b_src, dst_ap, b_dst, n, bank=None):
        op, ps, pb = self.op, self.ps, self.pb
        if bank is None:
            bank = n % 2
        pv = ps[bank][:].bitcast(BF16).rearrange("p (c t) -> p c t", c=8)
        for c in range(8):
            op("pe", lambda e, c=c, pv=pv: e.transpose(out=pv[:, c, :], in_=src[:, c * 128:(c + 1) * 128], identity=self.ident_b[:]),
               reads=[b_src, self.b_c], writes=[pb[bank]])
        if n % 2 == 0:
            op("act", lambda e, pv=pv: e.activation(out=dst_ap, in_=pv, func=AF.Copy), reads=[pb[bank]], writes=b_dst)
        else:
            op("dve", lambda e, pv=pv: e.tensor_copy(out=dst_ap, in_=pv), reads=[pb[bank]], writes=b_dst)

    def phase_1b(self, WhS, b_WhS, yaT_d, sub_g, nlam):
        nc, op, sb = self.nc, self.op, self.sb
        ps, pb, xnT, b_xnT, b_c = self.ps, self.pb, self.xnT, self.b_xnT, self.b_c
        qT = sb("qT", [128, S_LEN], BF16)
        kT = sb("kT", [128, S_LEN], BF16)
        Vh = sb("Vh", [128, NT, 130], BF16)
        b_qT = [self.B("qT%d" % i) for i in range(8)]
        b_kT = [self.B("kT%d" % i) for i in range(8)]
        b_V = [self.B("V%d" % i) for i in range(8)]
        Wh = [sb("Wh%d" % i, [128, 8, 384], BF16) for i in range(2)]
        b_Wh = [self.B("Wh%d" % i) for i in range(2)]
        PT = [sb("PT%d" % i, [128, 512], BF16) for i in range(4)]
        b_PT = [self.B("PT%d" % i) for i in range(4)]
        ya = [sb("ya%d" % i, [128, 4, 128], BF16) for i in range(2)]
        b_ya = [self.B("ya%d" % i) for i in range(2)]
        yaTs = [sb("yaTs%d" % i, [128, 512], BF16) for i in range(2)]
        b_yaTs = [self.B("yaTs%d" % i) for i in range(2)]
        ev = [sb("ev%d" % i, [128, 8], F32) for i in range(2)]
        b_ev = [self.B("ev%d" % i) for i in range(2)]
        o1t = [sb("o1t%d" % i, [128, 128], F32) for i in range(2)]
        o2t = [sb("o2t%d" % i, [128, 128], F32) for i in range(2)]
        osq = [sb("osq%d" % i, [128, 128], F32) for i in range(2)]
        b_ot = [self.B("ot%d" % i) for i in range(2)]
        self.b_yaT_d = Buf("yaT_d")
        eps = self.eps_ap()

        op("pool", lambda e: e.memset(Vh[:, :, 128:130], 1.0), writes=b_V)

        cp = [0]

        def evac_copy(out_ap, in_ap, reads, writes):
            cp[0] += 1
            if cp[0] % 2 == 0:
                op("act", lambda e: e.activation(out=out_ap, in_=in_ap, func=AF.Copy), reads=reads, writes=writes)
            else:
                op("dve", lambda e: e.tensor_copy(out=out_ap, in_=in_ap), reads=reads, writes=writes)

        evn = [0]
        Osb = [sb("Osb%d" % i, [128, 4, 258], F32) for i in range(2)]
        b_Osb = [self.B("Osb%d" % i) for i in range(2)]
        pending = [None]

        def emit_transposes(yi, h, qb):
            tb = 0
            pv = ps[tb][:].bitcast(BF16)[:, 0:512].rearrange("p (j t) -> p j t", j=4)
            for j in range(4):
                op("pe", lambda e, yi=yi, j=j, pv=pv: e.transpose(out=pv[:, j, :], in_=ya[yi][:, j, :], identity=self.ident_b[:]),
                   reads=[b_ya[yi], b_c], writes=[pb[tb]])
            evac_copy(yaTs[yi][:], ps[tb][:].bitcast(BF16)[:, 0:512], [pb[tb]], [b_yaTs[yi]])
            op("sp", lambda e, yi=yi, h=h, qb=qb: e.dma_start(out=yaT_d[h, :, qb * 512:(qb + 1) * 512], in_=yaTs[yi][:]),
               reads=[b_yaTs[yi]], writes=[self.b_yaT_d], dma=b_yaTs[yi])

        for h in range(self.lim.get('heads', 8)):
            wi = h % 2
            W = Wh[wi]
            op("sp", lambda e, h=h, W=W: e.dma_start(out=W[:].rearrange("p k c -> p (k c)"), in_=WhS[h]),
               reads=[b_WhS[h]], writes=[b_Wh[wi]], dma=b_Wh[wi])
            for which, dst, bdst in ((0, qT, b_qT), (1, kT, b_kT)):
                for nb in range(8):
                    bank = nb % 4
                    for kc in range(8):
                        op("pe", lambda e, kc=kc, nb=nb, bank=bank, which=which, W=W: e.matmul(
                            ps[bank][:, :], lhsT=W[:, kc, which * 128:(which + 1) * 128], rhs=xnT[:, kc, nb * 512:(nb + 1) * 512],
                            start=(kc == 0), stop=(kc == 7)),
                           reads=[b_Wh[wi]] + b_xnT[nb * 4:(nb + 1) * 4], writes=[pb[bank]])
                    evac_copy(dst[:, nb * 512:(nb + 1) * 512], ps[bank][:, :], [pb[bank]], [bdst[nb]])
            for grp in range(8):
                bank = grp % 4
                for t in range(4):
                    tt = grp * 4 + t
                    for kc in range(8):
                        op("pe", lambda e, kc=kc, tt=tt, t=t, bank=bank, W=W: e.matmul(
                            ps[bank][:, t * 128:(t + 1) * 128], lhsT=xnT[:, kc, tt * 128:(tt + 1) * 128], rhs=W[:, kc, 256:384],
                            start=(kc == 0), stop=(kc == 7)),
                           reads=[b_Wh[wi], b_xnT[tt]], writes=[pb[bank]])
                evac_copy(Vh[:, grp * 4:(grp + 1) * 4, 0:128], ps[bank][:, :].rearrange("p (t c) -> p t c", t=4), [pb[bank]], [b_V[grp]])
            self.convert_tables_part(h)
            for qb in range(self.lim.get('qbs', 8)):
                nkt = 4 * qb + 4
                def emit_S(kt):
                    jmin = max(0, kt - 4 * qb)
                    nq = 512 - 128 * jmin
                    q0 = qb * 512 + jmin * 128
                    for m in range(2):
                        sbank = m * 2 + (kt % 2)
                        op("pe", lambda e, m=m, kt=kt, sbank=sbank, nq=nq, q0=q0: e.matmul(
                            ps[sbank][:, 0:nq], lhsT=kT[m * 64:(m + 1) * 64, kt * 128:(kt + 1) * 128],
                            rhs=qT[m * 64:(m + 1) * 64, q0:q0 + nq], start=True, stop=True),
                           reads=[b_kT[kt // 4], b_qT[qb]], writes=[pb[sbank]])

                emit_S(0)
                for kt in range(nkt):
                    jmin = max(0, kt - 4 * qb)
                    nq = 512 - 128 * jmin
                    if kt + 1 < nkt:
                        emit_S(kt + 1)
                    for m in range(2):
                        sbank = m * 2 + (kt % 2)
                        r = (kt * 2 + m) % 4
                        op("act", lambda e, sbank=sbank, r=r, nq=nq: e.activation(out=PT[r][:, 0:nq], in_=ps[sbank][:, 0:nq], func=AF.Exp, scale=0.125),
                           reads=[pb[sbank]], writes=[b_PT[r]])
                        if kt >= 4 * qb:
                            op("dve", lambda e, r=r: e.memset(PT[r][64:128, 0:64], 0.0), writes=[b_PT[r]])
                    for m in range(2):
                        r = (kt * 2 + m) % 4
                        for j in range(jmin, 4):
                            obank = 4 + m * 2 + j // 2
                            off = (j % 2) * 129
                            first = (kt == 0 and j % 2 == 0)
                            op("pe", lambda e, r=r, j=j, jmin=jmin, kt=kt, obank=obank, off=off, first=first: e.matmul(
                                ps[obank][:, off:off + 129], lhsT=PT[r][:, (j - jmin) * 128:(j - jmin + 1) * 128], rhs=Vh[:, kt, 0:129],
                                start=first, stop=(kt == 4 * qb + j), skip_group_check=True),
                               reads=[b_PT[r], b_V[kt // 4]], writes=[pb[obank]])
                if pending[0] is not None:
                    emit_transposes(*pending[0])
                    pending[0] = None
                yi = (h * 8 + qb) % 2
                Os, b_Os = Osb[yi], b_Osb[yi]
                for b4 in range(4):
                    op("dve", lambda e, b4=b4, Os=Os: e.tensor_copy(out=Os[:, b4, :], in_=ps[4 + b4][:, 0:258]), reads=[pb[4 + b4]], writes=[b_Os])
                for j in range(4):
                    ei = evn[0] % 2
                    evn[0] += 1
                    e_t, o1, o2, sq = ev[ei], o1t[ei], o2t[ei], osq[ei]
                    off = (j % 2) * 129
                    b1, b2 = 4 + j // 2, 6 + j // 2
                    O1 = Os[:, b1 - 4, off:off + 129]
                    O2 = Os[:, b2 - 4, off:off + 129]
                    op("dve", lambda e, e_t=e_t, O1=O1: e.reciprocal(out=e_t[:, 0:1], in_=O1[:, 128:129]), reads=[b_Os], writes=[b_ev[ei]])
                    op("dve", lambda e, e_t=e_t, O2=O2: e.reciprocal(out=e_t[:, 1:2], in_=O2[:, 128:129]), reads=[b_Os], writes=[b_ev[ei]])
                    op("dve", lambda e, e_t=e_t: e.tensor_tensor(out=e_t[:, 2:3], in0=e_t[:, 1:2], in1=nlam[:], op=ALU.mult),
                       reads=[b_ev[ei], b_c], writes=[b_ev[ei]])
                    op("dve", lambda e, e_t=e_t, O1=O1, o1=o1: e.tensor_scalar(out=o1[:], in0=O1[:, 0:128], scalar1=e_t[:, 0:1], scalar2=None, op0=ALU.mult),
                       reads=[b_Os, b_ev[ei]], writes=[b_ot[ei]])
                    op("dve", lambda e, e_t=e_t, O2=O2, o1=o1, o2=o2: e.scalar_tensor_tensor(
                        out=o2[:], in0=O2[:, 0:128], scalar=e_t[:, 2:3], in1=o1[:], op0=ALU.mult, op1=ALU.add),
                       reads=[b_Os, b_ev[ei], b_ot[ei]], writes=[b_ot[ei]])
                    op("dve", lambda e, e_t=e_t, o2=o2, sq=sq: e.scalar_tensor_tensor(
                        out=sq[:], in0=o2[:], scalar=1.0, in1=o2[:], op0=ALU.mult, op1=ALU.mult, accum_out=e_t[:, 3:4]),
                       reads=[b_ot[ei]], writes=[b_ot[ei], b_ev[ei]])
                    op("act", lambda e, e_t=e_t: e.activation(out=e_t[:, 4:5], in_=e_t[:, 3:4], func=AF.Ln, scale=1.0 / 128, bias=eps),
                       reads=[b_ev[ei], b_c], writes=[b_ev[ei]])
                    op("act", lambda e, e_t=e_t: e.activation(out=e_t[:, 5:6], in_=e_t[:, 4:5], func=AF.Exp, scale=-0.5),
                       reads=[b_ev[ei]], writes=[b_ev[ei]])
                    op("dve", lambda e, e_t=e_t, o2=o2, yi=yi, j=j: e.scalar_tensor_tensor(
                        out=ya[yi][:, j, :], in0=o2[:], scalar=e_t[:, 5:6], in1=sub_g[:], op0=ALU.mult, op1=ALU.mult),
                       reads=[b_ot[ei], b_ev[ei], b_c], writes=[b_ya[yi]])
                pending[0] = (yi, h, qb)
        if pending[0] is not None:
            emit_transposes(*pending[0])
        if "yaT" in self.dbg:
            o = self.dout("dbg_yaT", [8, 128, S_LEN], BF16)
            for h in range(8):
                op("pool", lambda e, h=h: e.dma_start(out=o[h], in_=yaT_d[h]), reads=[self.b_yaT_d], writes=[self.b_out], dma=self.b_dbg)

    def phase_1c(self, WgS, b_WgS, WbrS, b_WbrS, yaT_d, lngb, lnbb, bsb, wmT):
        nc, op, sb, B = self.nc, self.op, self.sb, self.B
        ps, pb, xnT, b_xnT, b_c = self.ps, self.pb, self.xnT, self.b_xnT, self.b_c
        x, hbuf = self.x, self.hbuf
        NR = 4
        Wr = [sb("Wr%d" % i, [128, 8, 512], BF16) for i in range(NR)]
        b_Wr = [B("Wr%d" % i) for i in range(NR)]
        uT = sb("uT", [128, 8, 512], BF16); b_uT = B("uT")
        gT = sb("gT", [128, 16, 512], BF16); b_gT = B("gT")
        gvf = [sb("gvf%d" % i, [128, 1024], F32) for i in range(2)]
        b_gvf = [B("gvf%d" % i) for i in range(2)]
        lst = [sb("lst%d" % i, [128, 8], F32) for i in range(2)]
        b_lst = [B("lst%d" % i) for i in range(2)]
        vln = [sb("vln%d" % i, [128, 1024], BF16) for i in range(4)]
        b_vln = [B("vln%d" % i) for i in range(4)]
        yaTb = sb("yaTb", [128, 8, 512], BF16); b_yaTb = B("yaTb")
        ybT = sb("ybT", [128, 8, 512], BF16); b_ybT = B("ybT")
        mT = sb("mT", [128, 8, 512], BF16); b_mT = B("mT")
        t1 = [sb("t1_%d" % i, [128, 512], F32) for i in range(2)]
        t2 = [sb("t2_%d" % i, [128, 512], F32) for i in range(2)]
        b_t1 = [B("t1_%d" % i) for i in range(2)]
        b_t2 = [B("t2_%d" % i) for i in range(2)]
        xt = [sb("xt%d" % i, [128, 1024], F32) for i in range(2)]
        b_xt = [B("xt%d" % i) for i in range(2)]
        self.b_hbuf = Buf("hbuf")
        eps = self.eps_ap()

        nblk = self.lim.get("blocks", 8)
        seq = []
        for nb in range(nblk):
            seq += [(WgS, b_WgS, g) for g in range(8)]
            seq += [(WbrS, b_WbrS, g) for g in (0, 2, 1, 3, 4, 5)]
        issued = [0]

        def wslot(i, ahead=NR):
            while issued[0] < min(len(seq), i + ahead):
                k = issued[0]
                src, bsrc, g = seq[k]
                sl = k % NR
                op("sp", lambda e, src=src, g=g, sl=sl: e.dma_start(out=Wr[sl][:].rearrange("p k c -> p (k c)"), in_=src[g]),
                   reads=[bsrc], writes=[b_Wr[sl]], dma=b_Wr[sl])
                issued[0] += 1
            return i % NR

        bk = [0]

        def nbank():
            bk[0] = (bk[0] + 1) % 8
            return bk[0]

        wi = 0
        for nb in range(nblk):
            tok = slice(nb * 512, (nb + 1) * 512)
            bx = b_xnT[nb * 4:(nb + 1) * 4]
            op("sp", lambda e, nb=nb: e.dma_start(out=yaTb[:], in_=yaT_d[:, :, nb * 512:(nb + 1) * 512].rearrange("h p t -> p h t")),
               reads=[self.b_yaT_d], writes=[b_yaTb], dma=b_yaTb)
            for gi in range(2):
                sl = wslot(wi); wi += 1
                for c in range(4):
                    bank = nbank()
                    for kc in range(8):
                        op("pe", lambda e, kc=kc, c=c, sl=sl, bank=bank, tok=tok: e.matmul(
                            ps[bank][:, :], lhsT=Wr[sl][:, kc, c * 128:(c + 1) * 128], rhs=xnT[:, kc, tok], start=(kc == 0), stop=(kc == 7)),
                           reads=[b_Wr[sl]] + bx, writes=[pb[bank]])
                    op("act", lambda e, bank=bank, ch=gi * 4 + c: e.activation(out=uT[:, ch, :], in_=ps[bank][:, :], func=AF.Gelu),
                       reads=[pb[bank]], writes=[b_uT])
            sl0 = wslot(wi); sl1 = wslot(wi + 1, NR - 1); wi += 2
            for t in range(4):
                tt = nb * 4 + t
                gi_ = t % 2
                G, st_ = gvf[gi_], lst[gi_]
                for half, sl in ((0, sl0), (1, sl1)):
                    bank = nbank()
                    for kc in range(8):
                        op("pe", lambda e, kc=kc, sl=sl, bank=bank, tt=tt: e.matmul(
                            ps[bank][:, :], lhsT=xnT[:, kc, tt * 128:(tt + 1) * 128], rhs=Wr[sl][:, kc, :], start=(kc == 0), stop=(kc == 7)),
                           reads=[b_Wr[sl], b_xnT[tt]], writes=[pb[bank]])
                    op("act", lambda e, bank=bank, G=G, st_=st_, half=half: e.activation(
                        out=G[:, half * 512:(half + 1) * 512], in_=ps[bank][:, :], func=AF.Gelu, accum_out=st_[:, half:half + 1]),
                       reads=[pb[bank]], writes=[b_gvf[gi_], b_lst[gi_]])
                op("dve", lambda e, G=G, st_=st_, t=t: e.scalar_tensor_tensor(out=vln[t][:], in0=G[:], scalar=1.0, in1=G[:], op0=ALU.mult, op1=ALU.mult,
                                                                      accum_out=st_[:, 2:3]), reads=[b_gvf[gi_]], writes=[b_vln[t], b_lst[gi_]])
                op("dve", lambda e, st_=st_: e.tensor_tensor(out=st_[:, 3:4], in0=st_[:, 0:1], in1=st_[:, 1:2], op=ALU.add), reads=[b_lst[gi_]], writes=[b_lst[gi_]])
                op("dve", lambda e, st_=st_: e.tensor_scalar(out=st_[:, 3:4], in0=st_[:, 3:4], scalar1=1.0 / 1024, scalar2=None, op0=ALU.mult),
                   reads=[b_lst[gi_]], writes=[b_lst[gi_]])
                op("dve", lambda e, st_=st_: e.tensor_tensor(out=st_[:, 4:5], in0=st_[:, 3:4], in1=st_[:, 3:4], op=ALU.mult), reads=[b_lst[gi_]], writes=[b_lst[gi_]])
                op("dve", lambda e, st_=st_: e.scalar_tensor_tensor(out=st_[:, 5:6], in0=st_[:, 2:3], scalar=1.0 / 1024, in1=st_[:, 4:5],
                                                                 op0=ALU.mult, op1=ALU.subtract), reads=[b_lst[gi_]], writes=[b_lst[gi_]])
                op("act", lambda e, st_=st_: e.activation(out=st_[:, 6:7], in_=st_[:, 5:6], func=AF.Sqrt, bias=eps), reads=[b_lst[gi_], b_c], writes=[b_lst[gi_]])
                op("dve", lambda e, st_=st_: e.reciprocal(out=st_[:, 7:8], in_=st_[:, 6:7]), reads=[b_lst[gi_]], writes=[b_lst[gi_]])
                op("dve", lambda e, G=G, st_=st_: e.tensor_scalar(out=G[:], in0=G[:], scalar1=st_[:, 3:4], scalar2=st_[:, 7:8], op0=ALU.subtract, op1=ALU.mult),
                   reads=[b_gvf[gi_], b_lst[gi_]], writes=[b_gvf[gi_]])
                op("dve", lambda e, G=G: e.tensor_tensor(out=G[:], in0=G[:], in1=lngb[:], op=ALU.mult), reads=[b_gvf[gi_], b_c], writes=[b_gvf[gi_]])
                op("dve", lambda e, G=G, t=t: e.tensor_tensor(out=vln[t][:], in0=G[:], in1=lnbb[:], op=ALU.add), reads=[b_gvf[gi_], b_c], writes=[b_vln[t]])
            for gi in range(4):
                sl = wslot(wi); wi += 1
                for c in range(4):
                    bank = nbank()
                    for kc in range(8):
                        op("pe", lambda e, kc=kc, c=c, sl=sl, bank=bank, tok=tok: e.matmul(
                            ps[bank][:, :], lhsT=Wr[sl][:, kc, c * 128:(c + 1) * 128], rhs=xnT[:, kc, tok], start=(kc == 0), stop=(kc == 7)),
                           reads=[b_Wr[sl]] + bx, writes=[pb[bank]])
                    op("act", lambda e, bank=bank, ch=gi * 4 + c: e.activation(out=gT[:, ch, :], in_=ps[bank][:, :], func=AF.Sigmoid),
                       reads=[pb[bank]], writes=[b_gT])
            for g in range(8):
                bank = nbank()
                for t in range(4):
                    op("pe", lambda e, g=g, t=t, bank=bank: e.matmul(
                        ps[bank][:, t * 128:(t + 1) * 128], lhsT=vln[t][:, g * 128:(g + 1) * 128], rhs=wmT[:, g, :], start=True, stop=True),
                       reads=[b_vln[t], b_c], writes=[pb[bank]])
                i2 = g % 2
                op("dve", lambda e, g=g, bank=bank, i2=i2: e.tensor_tensor(
                    out=t1[i2][:].rearrange("p (t i) -> p t i", t=4), in0=ps[bank][:, :].rearrange("p (t i) -> p t i", t=4),
                    in1=bc(bsb[:, g * 128:(g + 1) * 128], 0, 4), op=ALU.add), reads=[pb[bank], b_c], writes=[b_t1[i2]])
                op("dve", lambda e, g=g, i2=i2: e.tensor_tensor(out=ybT[:, g, :], in0=t1[i2][:], in1=uT[:, g, :], op=ALU.mult),
                   reads=[b_t1[i2], b_uT], writes=[b_ybT])
            for gi in range(2):
                slA = wslot(wi); slB = wslot(wi + 1, NR - 1); wi += 2
                for c in range(4):
                    fo = gi * 4 + c
                    i2 = fo % 2
                    bankA = nbank()
                    for kc in range(8):
                        op("pe", lambda e, kc=kc, c=c, slA=slA, bankA=bankA: e.matmul(
                            ps[bankA][:, :], lhsT=Wr[slA][:, kc, c * 128:(c + 1) * 128], rhs=yaTb[:, kc, :], start=(kc == 0), stop=(kc == 7)),
                           reads=[b_Wr[slA], b_yaTb], writes=[pb[bankA]])
                    bankB = nbank()
                    for kc in range(8):
                        op("pe", lambda e, kc=kc, c=c, slB=slB, bankB=bankB: e.matmul(
                            ps[bankB][:, :], lhsT=Wr[slB][:, kc, c * 128:(c + 1) * 128], rhs=ybT[:, kc, :], start=(kc == 0), stop=(kc == 7)),
                           reads=[b_Wr[slB], b_ybT], writes=[pb[bankB]])
                    op("dve", lambda e, bankA=bankA, fo=fo, i2=i2: e.tensor_tensor(out=t1[i2][:], in0=ps[bankA][:, :], in1=gT[:, fo, :], op=ALU.mult),
                       reads=[pb[bankA], b_gT], writes=[b_t1[i2]])
                    op("dve", lambda e, bankB=bankB, fo=fo, i2=i2: e.tensor_tensor(out=t2[i2][:], in0=ps[bankB][:, :], in1=gT[:, 8 + fo, :], op=ALU.mult),
                       reads=[pb[bankB], b_gT], writes=[b_t2[i2]])
                    op("dve", lambda e, fo=fo, i2=i2: e.tensor_tensor(out=mT[:, fo, :], in0=t1[i2][:], in1=t2[i2][:], op=ALU.add),
                       reads=[b_t1[i2], b_t2[i2]], writes=[b_mT])
            slo0 = wslot(wi); slo1 = wslot(wi + 1, NR - 1); wi += 2
            for t in range(4):
                tt = nb * 4 + t
                i2 = t % 2
                op("sp", lambda e, tt=tt, i2=i2: e.dma_start(out=xt[i2][:], in_=x[tt * 128:(tt + 1) * 128, :]), writes=[b_xt[i2]], dma=b_xt[i2])
                for half, sl in ((0, slo0), (1, slo1)):
                    bank = nbank()
                    for kc in range(8):
                        op("pe", lambda e, kc=kc, sl=sl, bank=bank, t=t: e.matmul(
                            ps[bank][:, :], lhsT=mT[:, kc, t * 128:(t + 1) * 128], rhs=Wr[sl][:, kc, :], start=(kc == 0), stop=(kc == 7)),
                           reads=[b_Wr[sl], b_mT], writes=[pb[bank]])
                    op("dve", lambda e, bank=bank, half=half, i2=i2: e.tensor_tensor(
                        out=xt[i2][:, half * 512:(half + 1) * 512], in0=ps[bank][:, :], in1=xt[i2][:, half * 512:(half + 1) * 512], op=ALU.add),
                       reads=[pb[bank], b_xt[i2]], writes=[b_xt[i2]])
                op("sp", lambda e, tt=tt, i2=i2: e.dma_start(out=hbuf[tt * 128:(tt + 1) * 128, :], in_=xt[i2][:]),
                   reads=[b_xt[i2]], writes=[self.b_hbuf], dma=b_xt[i2])
        if "h" in self.dbg:
            o = self.dout("dbg_h", [S_LEN, D], F32)
            for q in range(8):
                op("pool", lambda e, q=q: e.dma_start(out=o[q * 512:(q + 1) * 512, :], in_=hbuf[q * 512:(q + 1) * 512, :]),
                   reads=[self.b_hbuf], writes=[self.b_out], dma=self.b_dbg)

    def phase_2(self):
        nc, op, sb, B = self.nc, self.op, self.sb, self.B
        ps, pb, b_c = self.ps, self.pb, self.b_c
        p2 = self.p2
        hbuf, out = self.hbuf, self.out
        vecs, iota_d = self.vecs, self.iota_d
        n2gb = sb("n2gb", [128, 1024], F32)
        fgb = sb("fgb", [128, 1024], F32)
        iota16 = sb("iota16", [128, 2048], F32)
        iota128f = sb("iota128f", [128, 128], F32)
        iota128 = sb("iota128", [128, 128], BF16)
        skT_f = sb("skT_f", [128, 256], F32)
        skTb = sb("skTb", [128, 2, 128], BF16)
        b_p2 = B("par2")
        for dst, src in ((n2gb[:], pbc(vecs[3:4, :])), (fgb[:], pbc(vecs[4:5, :])), (iota16[:], iota_d[:, 0:2048]),
                         (iota128f[:], iota_d[:, 2048:2176]), (skT_f[:], self.skT_d)):
            op("sp", lambda e, dst=dst, src=src: e.dma_start(out=dst, in_=src), writes=[b_p2], dma=b_p2)
        op("dve", lambda e: e.tensor_copy(out=iota128[:], in_=iota128f[:]), reads=[b_p2], writes=[b_p2])
        op("dve", lambda e: e.tensor_copy(out=skTb[:].rearrange("p a n -> p (a n)"), in_=skT_f[:]), reads=[b_p2], writes=[b_p2])
        b_c2 = b_p2
        self.rcst = sb("rcst", [128, 2], F32)
        self.b_rcst = B("rcst")
        op("dve", lambda e: e.memset(self.rcst[:, 0:1], float(D * EPS)), writes=[self.b_rcst])
        op("dve", lambda e: e.memset(self.rcst[:, 1:2], -0.5), writes=[self.b_rcst])
        TP = 256
        nblk = self.lim.get("pblocks", S_LEN // TP)
        NCH = self.lim.get("chunks", 128)
        self.alloc_rms()
        xnb, b_xnb = self.rms_tmp[2], self.rms_tmp[3]
        xtp = [self.xt, [sb("xtB%d" % i, [128, 1024], F32) for i in range(2)]]
        b_xtp = [self.b_xt, [B("xtB%d" % i) for i in range(2)]]
        xn2Tp = [sb("xn2T%d" % i, [128, 8, TP], BF16) for i in range(3)]
        b_xn2Tp = [B("xn2T%d" % i) for i in range(3)]
        pqT = sb("pqT", [128, 16, TP], BF16); b_pqT = B("pqT")
        Wq = [sb("Wq%d" % i, [128, 8, 256], BF16) for i in range(2)]
        b_Wq = [B("Wq%d" % i) for i in range(2)]
        sc = sb("sc", [128, 16, 128], F32); b_sc = [B("sc%d" % i) for i in range(16)]
        svt = sb("svt", [128, 16, 16], F32)
        sit = sb("sit", [128, 16, 16], U32); b_sv = [B("sv%d" % i) for i in range(16)]
        cand = sb("cand", [128, 8, 256], F32); b_cand = [B("cand%d" % i) for i in range(8)]
        ts = sb("ts", [128, 8, 16], F32)
        pos = sb("pos", [128, 8, 16], U32); b_ts = [B("ts%d" % i) for i in range(8)]
        ee = sb("ee", [128, 8, 16], F32)
        zz = sb("zz", [128, 16], F32)
        gg = sb("gg", [128, 8, 16], BF16); b_g = B("g")
        pa = sb("pa", [128, 128], U32)
        pbb = sb("pbb", [128, 128], U32)
        af = sb("af", [128, 8, 16], F32)
        bf_ = sb("bf_", [128, 8, 16], F32)
        sif = sb("sif", [128, 16, 16], F32); b_ix = B("ix")
        II = sb("II", [128, 128], BF16)
        JJ = sb("JJ", [128, 128], BF16); b_IJ = B("IJ")
        IJGp = [sb("IJG%d" % i, [128, 3, TP], BF16) for i in range(2)]
        b_IJGp = [B("IJG%d" % i) for i in range(2)]
        SBK = 16
        A = [sb("A%d" % i, [128, SBK, 128], BF16) for i in range(2)]
        Bm = [sb("Bm%d" % i, [128, SBK, 128], BF16) for i in range(2)]
        b_A = [B("A%d" % i) for i in range(2)]
        b_Bm = [B("Bm%d" % i) for i in range(2)]
        CL = 60
        WH = 128 - CL
        WsbL = sb("WsbL", [128, TP, CL], BF16); b_WsbL = B("WsbL")
        WsbH = sb("WsbH", [128, TP, WH], BF16); b_WsbH = B("WsbH")
        stg = [sb("stg%d" % i, [128, SBK, WH], BF16) for i in range(2)]
        b_stg = [B("stg%d" % i) for i in range(2)]
        WH_d = self.dscr("WH_d", [2, 128, TP * WH], BF16)
        b_WHd = [B("WHd%d" % i) for i in range(2)]
        NRING = 6
        UT = [sb("UT%d" % i, [128, 8, 128], BF16) for i in range(NRING)]
        Vt = [sb("Vt%d" % i, [128, 1024], BF16) for i in range(NRING)]
        b_UT = [B("UT%d" % i) for i in range(NRING)]
        b_Vt = [B("Vt%d" % i) for i in range(NRING)]
        Gt = [sb("Gt%d" % i, [128, TP], BF16) for i in range(3)]
        Mt = [sb("Mt%d" % i, [128, TP], BF16) for i in range(3)]
        b_Gt = [B("Gt%d" % i) for i in range(3)]
        b_Mt = [B("Mt%d" % i) for i in range(3)]

        bk = [0]

        def nbank():
            bk[0] = (bk[0] + 1) % 4
            return bk[0]

        cp = [0]

        def evac_copy(out_ap, in_ap, reads, writes, act_share=3):
            cp[0] += 1
            if cp[0] % 3 < act_share:
                op("act", lambda e: e.activation(out=out_ap, in_=in_ap, func=AF.Copy), reads=reads, writes=writes)
            else:
                op("dve", lambda e: e.tensor_copy(out=out_ap, in_=in_ap), reads=reads, writes=writes)

        def top16(srcs, vals, idxs, b_srcs, b_dsts, every=8, pad=0):
            n = len(srcs)

            def brk(k):
                if k % every == every - 1:
                    for _ in range(1 + pad):
                        yield

            for r in range(2):
                sl = slice(r * 8, (r + 1) * 8)
                for k in range(n):
                    op("dve", lambda e, k=k, sl=sl: e.max(out=vals[k][:, sl], in_=srcs[k]), reads=[b_srcs[k]], writes=[b_dsts[k]])
                    yield from brk(k)
                for k in range(n):
                    op("dve", lambda e, k=k, sl=sl: e.max_index(out=idxs[k][:, sl], in_max=vals[k][:, sl], in_values=srcs[k]),
                       reads=[b_srcs[k], b_dsts[k]], writes=[b_dsts[k]])
                    yield from brk(k)
                if r == 0:
                    for k in range(n):
                        op("dve", lambda e, k=k, sl=sl: e.match_replace(out=srcs[k], in_to_replace=vals[k][:, sl], in_values=srcs[k], imm_value=NEG),
                           reads=[b_srcs[k], b_dsts[k]], writes=[b_srcs[k]])
                        yield from brk(k)

        total = nblk * NCH
        issued = [0]

        def tload(upto, oldest):
            while issued[0] < min(total, upto + 1, oldest + NRING):
                k = issued[0]
                i = k % NCH
                sl = k % NRING
                op("sp", lambda e, i=i, sl=sl: e.dma_start(out=UT[sl][:].rearrange("p k j -> p (k j)"), in_=p2["UTS"][i]),
                   reads=[self.b_UTS], writes=[b_UT[sl]], dma=b_UT[sl])
                op("sp", lambda e, i=i, sl=sl: e.dma_start(out=Vt[sl][:], in_=p2["VS"][i]),
                   reads=[self.b_VS], writes=[b_Vt[sl]], dma=b_Vt[sl])
                issued[0] += 1

        def pro_AB(blk):
            xt, b_xt, xn2T, b_xn2T = xtp[0], b_xtp[0], xn2Tp[blk % 3], b_xn2Tp[blk % 3]
            wq4 = p2["wqS"].rearrange("g p (k c) -> g p k c", k=8)

            def wq_load(hg):
                op("sp", lambda e, hg=hg: e.dma_start(out=Wq[hg % 2][:], in_=wq4[hg // 2, :, :, (hg % 2) * 256:(hg % 2 + 1) * 256]),
                   reads=[self.b_wqS], writes=[b_Wq[hg % 2]], dma=b_Wq[hg % 2])

            for t in range(2):
                tt = blk * 2 + t
                op("sp", lambda e, tt=tt, t=t: e.dma_start(out=xt[t][:], in_=hbuf[tt * 128:(tt + 1) * 128, :]),
                   reads=[self.b_hbuf], writes=[b_xt[t]], dma=b_xt[t])
            wq_load(0)
            for _ in range(4):
                yield
            for t in range(2):
                self.rms_sq(xt[t], b_xt[t], t)
            yield
            for t in range(2):
                tt = blk * 2 + t
                self.rms_fin(xt[t], b_xt[t], n2gb, t, b_g=b_p2)
                yield
                self.transpose_tile(xnb[t], b_xnb[t], xn2T[:, :, t * 128:(t + 1) * 128], [b_xn2T], tt, bank=3)
            yield
            for hg in range(8):
                sl = hg % 2
                if hg + 1 < 8:
                    wq_load(hg + 1)
                for c in range(2):
                    bank = 2
                    for kc in range(8):
                        op("pe", lambda e, kc=kc, c=c, sl=sl, bank=bank: e.matmul(
                            ps[bank][:, 0:TP], lhsT=Wq[sl][:, kc, c * 128:(c + 1) * 128], rhs=xn2T[:, kc, :], start=(kc == 0), stop=(kc == 7)),
                           reads=[b_Wq[sl], b_xn2T], writes=[pb[bank]])
                    evac_copy(pqT[:, hg * 2 + c, :], ps[bank][:, 0:TP], [pb[bank]], [b_pqT])
                    yield
            yield

        def pro_chain(blk):
            par = blk % 2
            IJG, b_IJG = IJGp[par], b_IJGp[par]
            for t in range(2):
                for bq in range(4):
                    bank = 3
                    for k in range(4):
                        hp = bq * 4 + k
                        op("pe", lambda e, hp=hp, k=k, t=t, bank=bank: e.matmul(
                            ps[bank][:, k * 128:(k + 1) * 128], lhsT=pqT[:, hp, t * 128:(t + 1) * 128], rhs=skTb[:, hp % 2, :], start=True, stop=True),
                           reads=[b_pqT, b_c2], writes=[pb[bank]])
                    evac_copy(sc[:, bq * 4:(bq + 1) * 4, :], ps[bank][:, :].rearrange("p (a n) -> p a n", a=4), [pb[bank]], b_sc[bq * 4:(bq + 1) * 4])
                    yield
                yield
                yield from top16([sc[:, hp, :] for hp in range(16)], [svt[:, hp, :] for hp in range(16)], [sit[:, hp, :] for hp in range(16)], b_sc, b_sv,
                                 every=8, pad=1)
                svv = svt[:].rearrange("p (h two) k -> p h two k", two=2)
                op("dve", lambda e, svv=svv: e.tensor_tensor(out=cand[:].rearrange("p h (a b) -> p h a b", a=16),
                                                             in0=bc(svv[:, :, 0, :], 2, 16), in1=bc(svv[:, :, 1, :], 1, 16), op=ALU.add),
                   reads=b_sv, writes=b_cand)
                yield
                yield
                yield from top16([cand[:, h, :] for h in range(8)], [ts[:, h, :] for h in range(8)], [pos[:, h, :] for h in range(8)], b_cand, b_ts,
                                 every=4, pad=0)
                op("dve", lambda e: e.tensor_tensor(out=ee[:], in0=ts[:], in1=bc(ts[:, :, 0], 1, 16), op=ALU.subtract), reads=b_ts, writes=[b_g])
                yield
                op("act", lambda e: e.activation(out=ee[:], in_=ee[:], func=AF.Exp), reads=[b_g], writes=[b_g])
                yield
                op("dve", lambda e: e.tensor_reduce(out=zz[:, 0:8], in_=ee[:], axis=AX.X, op=ALU.add), reads=[b_g], writes=[b_g])
                op("dve", lambda e: e.reciprocal(out=zz[:, 8:16], in_=zz[:, 0:8]), reads=[b_g], writes=[b_g])
                op("dve", lambda e: e.tensor_tensor(out=gg[:], in0=ee[:], in1=bc(zz[:, 8:16], 1, 16), op=ALU.mult), reads=[b_g], writes=[b_g])
                pos2 = pos[:].rearrange("p h k -> p (h k)")
                op("dve", lambda e, pos2=pos2: e.tensor_single_scalar(out=pa[:], in_=pos2, scalar=4, op=ALU.logical_shift_right), reads=b_ts, writes=[b_ix])
                op("dve", lambda e, pos2=pos2: e.tensor_single_scalar(out=pbb[:], in_=pos2, scalar=15, op=ALU.bitwise_and), reads=b_ts, writes=[b_ix])
                op("dve", lambda e: e.tensor_copy(out=af[:].rearrange("p h k -> p (h k)"), in_=pa[:]), reads=[b_ix], writes=[b_ix])
                op("dve", lambda e: e.tensor_copy(out=bf_[:].rearrange("p h k -> p (h k)"), in_=pbb[:]), reads=[b_ix], writes=[b_ix])
                op("dve", lambda e: e.tensor_copy(out=sif[:], in_=sit[:]), reads=b_sv, writes=[b_ix])
                yield
                sifv = sif[:].rearrange("p (h two) k -> p h two k", two=2)
                eq4 = cand[:].rearrange("p h (a b) -> p h a b", a=16)
                io4 = iota16[:].rearrange("p (h a b) -> p h a b", h=8, a=16)
                for which, sel, dst in ((0, af, II), (1, bf_, JJ)):
                    op("dve", lambda e, sel=sel: e.tensor_tensor(out=eq4, in0=bc(sel[:], 2, 16), in1=io4, op=ALU.is_equal),
                       reads=[b_ix, b_c2] + b_ts, writes=b_cand)
                    yield
                    op("dve", lambda e, which=which: e.tensor_tensor(out=eq4, in0=eq4, in1=bc(sifv[:, :, which, :], 1, 16), op=ALU.mult),
                       reads=b_cand + [b_ix], writes=b_cand)
                    yield
                    def red(e, dst=dst):
                        with self.nc.allow_low_precision("sum of one-hot * small integer index: exact in bf16"):
                            return e.tensor_reduce(out=dst[:].rearrange("p (h k) -> p h k", h=8), in_=eq4, axis=AX.X, op=ALU.add)
                    op("dve", red, reads=b_cand, writes=[b_IJ])
                    yield
                yield
                bank = 3
                for n_, src in enumerate((II[:], JJ[:], gg[:].rearrange("p h k -> p (h k)"))):
                    rd = [b_IJ, b_c] if n_ < 2 else [b_g, b_c]
                    op("pe", lambda e, n_=n_, src=src, bank=bank: e.transpose(out=ps[bank][:].bitcast(BF16)[:, n_ * 128:(n_ + 1) * 128], in_=src, identity=self.ident_b[:]),
                       reads=rd, writes=[pb[bank]])
                evac_copy(IJG[:, :, t * 128:(t + 1) * 128], ps[bank][:].bitcast(BF16)[:, 0:384].rearrange("p (a n) -> p a n", a=3), [pb[bank]], [b_IJG])
                yield

        jkc = [0]

        def stage_JK(blk, banks):
            par = blk % 2
            IJG, b_IJG = IJGp[par], b_IJGp[par]
            WHv = WH_d[par].rearrange("p (t i) -> p t i", i=WH)
            io3 = bc(iota128[:], 0, SBK)

            def emit_dve(sbk, ai):
                t0 = sbk * SBK
                for tk in range(SBK):
                    tok = t0 + tk
                    op("dve", lambda e, ai=ai, tk=tk, tok=tok: e.tensor_scalar(
                        out=A[ai][:, tk, :], in0=iota128[:], scalar1=IJG[:, 0, tok:tok + 1], scalar2=IJG[:, 2, tok:tok + 1],
                        op0=ALU.is_equal, op1=ALU.mult), reads=[b_IJG, b_c2], writes=[b_A[ai]])
                op("dve", lambda e, ai=ai, t0=t0: e.tensor_tensor(out=Bm[ai][:], in0=io3, in1=bc(IJG[:, 1, t0:t0 + SBK], 1, 128), op=ALU.is_equal),
                   reads=[b_IJG, b_c2], writes=[b_Bm[ai]])

            def emit_group(sbk, ai, q4):
                jkc[0] += 1
                bank = banks[jkc[0] % len(banks)]
                for k in range(4):
                    tk = q4 * 4 + k
                    op("pe", lambda e, ai=ai, tk=tk, k=k, bank=bank: e.matmul(
                        ps[bank][:, k * 128:(k + 1) * 128], lhsT=Bm[ai][:, tk, :], rhs=A[ai][:, tk, :], start=True, stop=True),
                       reads=[b_A[ai], b_Bm[ai]], writes=[pb[bank]])
                pv = ps[bank][:, :].rearrange("p (t i) -> p t i", t=4)
                tok0 = sbk * SBK + q4 * 4
                r = sbk % 2
                op("act", lambda e, pv=pv, tok0=tok0: e.activation(out=WsbL[:, tok0:tok0 + 4, :], in_=pv[:, :, 0:CL], func=AF.Copy),
                   reads=[pb[bank]], writes=[b_WsbL])
                op("act", lambda e, pv=pv, r=r, q4=q4: e.activation(out=stg[r][:, q4 * 4:q4 * 4 + 4, :], in_=pv[:, :, CL:128], func=AF.Copy),
                   reads=[pb[bank]], writes=[b_stg[r]])

            def emit_spill(sbk):
                r = sbk % 2
                t0 = sbk * SBK
                op("sp", lambda e, r=r, t0=t0: e.dma_start(out=WHv[:, t0:t0 + SBK, :], in_=stg[r][:]),
                   reads=[b_stg[r]], writes=[b_WHd[par]], dma=b_WHd[par])

            nsb = TP // SBK
            emit_dve(0, 0)
            yield
            for sbk in range(nsb):
                for q4 in range(SBK // 4):
                    if q4 == 0 and sbk + 1 < nsb:
                        emit_dve(sbk + 1, (sbk + 1) % 2)
                    if q4 == 1 and sbk > 0:
                        emit_spill(sbk - 1)
                    emit_group(sbk, sbk % 2, q4)
                    yield
            emit_spill(nsb - 1)
            yield

        def fill(blk):
            par = blk % 2
            WHv = WH_d[par].rearrange("p (t i) -> p t i", i=WH)
            for sbk in range(TP // SBK):
                t0 = sbk * SBK
                op("sp", lambda e, t0=t0: e.dma_start(out=WsbH[:, t0:t0 + SBK, :], in_=WHv[:, t0:t0 + SBK, :]),
                   reads=[b_WHd[par]], writes=[b_WsbH], dma=b_WsbH)
                yield

        def step(g, n=1):
            if g is not None:
                for _ in range(n):
                    next(g, None)

        def flush(g):
            if g is not None:
                for _ in g:
                    pass

        def main_loop(blk, gF, gC, gL, gAB, gE):
            xn2T, b_xn2T = xn2Tp[blk % 3], b_xn2Tp[blk % 3]
            def emit_H(i):
                n = blk * NCH + i
                tload(n + 3, blk * NCH + max(0, i - 2))
                sl = n % NRING
                hb = n % 2
                for kc in range(8):
                    op("pe", lambda e, kc=kc, sl=sl, hb=hb: e.matmul(
                        ps[hb][:, 0:TP], lhsT=UT[sl][:, kc, :], rhs=xn2T[:, kc, :], start=(kc == 0), stop=(kc == 7)),
                       reads=[b_UT[sl], b_xn2T], writes=[pb[hb]])

            def emit_GM(i):
                n = blk * NCH + i
                hb = n % 2
                r3 = n % 3
                op("act", lambda e, hb=hb, r3=r3: e.activation(out=Gt[r3][:], in_=ps[hb][:, 0:TP], func=AF.Gelu), reads=[pb[hb]], writes=[b_Gt[r3]])
                Wv, b_Wv = (WsbL[:, :, i], b_WsbL) if i < CL else (WsbH[:, :, i - CL], b_WsbH)
                op("pool", lambda e, r3=r3, Wv=Wv: e.tensor_tensor(out=Mt[r3][:], in0=Gt[r3][:], in1=Wv, op=ALU.mult),
                   reads=[b_Gt[r3], b_Wv], writes=[b_Mt[r3]])

            emit_H(0)
            if NCH > 1:
                emit_H(1)
            emit_GM(0)
            for i in range(NCH):
                n = blk * NCH + i
                sl = n % NRING
                r3 = n % 3
                if i == 2:
                    flush(gE)
                if i == CL - 1:
                    flush(gF)
                    flush(gC)
                if i == NCH - 16:
                    for t in range(2):
                        tt = blk * 2 + t
                        op("sp", lambda e, tt=tt, t=t: e.dma_start(out=xtp[1][t][:], in_=hbuf[tt * 128:(tt + 1) * 128, :]),
                           reads=[self.b_hbuf], writes=[b_xtp[1][t]], dma=b_xtp[1][t])
                if i + 2 < NCH:
                    emit_H(i + 2)
                if i + 1 < NCH:
                    emit_GM(i + 1)
                if i < CL - 1:
                    step(gF)
                    if i < 2:
                        step(gE)
                    step(gC, 2)
                else:
                    step(gL)
                    if (i - CL) % 2 == 1:
                        step(gAB)
                for t in range(2):
                    for half in range(2):
                        yb_ = 4 + t * 2 + half
                        op("pe", lambda e, t=t, half=half, yb_=yb_, r3=r3, sl=sl, i=i: e.matmul(
                            ps[yb_][:, :], lhsT=Mt[r3][:, t * 128:(t + 1) * 128], rhs=Vt[sl][:, half * 512:(half + 1) * 512],
                            start=(i == 0), stop=(i == NCH - 1)),
                           reads=[b_Mt[r3], b_Vt[sl]], writes=[pb[yb_]])
            flush(gE)
            flush(gF)
            flush(gC)
            flush(gL)
            flush(gAB)

        def epilogue(blk):
            xt, b_xt = xtp[1], b_xtp[1]
            for t in range(2):
                for half in range(2):
                    yb_ = 4 + t * 2 + half
                    op("dve", lambda e, t=t, half=half, yb_=yb_: e.tensor_tensor(
                        out=xt[t][:, half * 512:(half + 1) * 512], in0=ps[yb_][:, :], in1=xt[t][:, half * 512:(half + 1) * 512], op=ALU.add),
                       reads=[pb[yb_], b_xt[t]], writes=[b_xt[t]])
            yield
            for t in range(2):
                self.rms_sq(xt[t], b_xt[t], t)
            yield
            for t in range(2):
                tt = blk * 2 + t
                self.rms_fin(xt[t], b_xt[t], fgb, t, out_ap=xt[t][:], b_o=b_xt[t], b_g=b_p2)
                op("sp", lambda e, tt=tt, t=t: e.dma_start(out=out[tt * 128:(tt + 1) * 128, :], in_=xt[t][:]),
                   reads=[b_xt[t]], writes=[self.b_out], dma=b_xt[t])
            yield

        flush(pro_AB(0))
        flush(pro_chain(0))
        flush(stage_JK(0, (0, 1, 2, 3)))
        if nblk > 1:
            flush(pro_AB(1))
        gE = None
        for blk in range(nblk):
            main_loop(blk, fill(blk),
                      pro_chain(blk + 1) if blk + 1 < nblk else None,
                      stage_JK(blk + 1, (3,)) if blk + 1 < nblk else None,
                      pro_AB(blk + 2) if blk + 2 < nblk else None,
                      gE)
            gE = epilogue(blk)
            next(gE)
        flush(gE)

    def rmsnorm_tile(self, xt, b_xt, gb, i, tag, out_ap=None, b_o=None, b_g=None):
        op = self.op
        junk, b_junk, xnb, b_xnb, st, b_st = self.rms_tmp
        s = st[i]
        if out_ap is None:
            out_ap, b_o = xnb[i][:], b_xnb[i]
        op("act", lambda e: e.activation(out=junk[:], in_=xt[:], func=AF.Square, accum_out=s[:, 0:1]),
           reads=[b_xt], writes=[b_junk, b_st[i]])
        op("act", lambda e: e.activation(out=s[:, 1:2], in_=s[:, 0:1], func=AF.Sqrt, scale=1.0 / D, bias=self.eps_ap()),
           reads=[b_st[i], self.b_c], writes=[b_st[i]])
        op("dve", lambda e: e.reciprocal(out=s[:, 2:3], in_=s[:, 1:2]), reads=[b_st[i]], writes=[b_st[i]])
        op("dve", lambda e: e.scalar_tensor_tensor(out=out_ap, in0=xt[:], scalar=s[:, 2:3], in1=gb[:], op0=ALU.mult, op1=ALU.mult),
           reads=[b_xt, b_st[i], self.b_c] + ([b_g] if b_g is not None else []), writes=[b_o])

    def rms_sq(self, xt, b_xt, i):
        junk, b_junk, xnb, b_xnb, st, b_st = self.rms_tmp
        s = st[i]
        self.op("act", lambda e: e.activation(out=junk[:], in_=xt[:], func=AF.Square, accum_out=s[:, 0:1]),
                reads=[b_xt], writes=[b_junk, b_st[i]])

    def rms_fin(self, xt, b_xt, gb, i, out_ap=None, b_o=None, b_g=None):
        op = self.op
        junk, b_junk, xnb, b_xnb, st, b_st = self.rms_tmp
        s = st[i]
        if out_ap is None:
            out_ap, b_o = xnb[i][:], b_xnb[i]
        cst = self.rcst
        op("pool", lambda e: e.tensor_tensor(out=s[:, 1:2], in0=s[:, 0:1], in1=cst[:, 0:1], op=ALU.add),
           reads=[b_st[i], self.b_rcst], writes=[b_st[i]])
        op("pool", lambda e: e.tensor_tensor(out=s[:, 3:4], in0=s[:, 1:2], in1=cst[:, 1:2], op=ALU.pow),
           reads=[b_st[i], self.b_rcst], writes=[b_st[i]])
        op("dve", lambda e: e.tensor_scalar(out=s[:, 2:3], in0=s[:, 3:4], scalar1=float(D ** 0.5), scalar2=None, op0=ALU.mult),
           reads=[b_st[i]], writes=[b_st[i]])
        op("dve", lambda e: e.scalar_tensor_tensor(out=out_ap, in0=xt[:], scalar=s[:, 2:3], in1=gb[:], op0=ALU.mult, op1=ALU.mult),
           reads=[b_xt, b_st[i], self.b_c] + ([b_g] if b_g is not None else []), writes=[b_o])

    def eps_ap(self):
        return self._eps[:]

    def finish(self, out):
        self.S.finish_waits("sp", [self.b_out, self.b_dbg])
        return self.S.emit()


def _arr_kc(w, ncols_group):
    C = w.shape[1]
    G = C // ncols_group
    a = w.reshape(8, 128, G, ncols_group).transpose(2, 1, 0, 3)
    return np.ascontiguousarray(a).reshape(G, 128, 8 * ncols_group)


def prep_shared(inp):
    f = np.float32
    w_in = np.asarray(inp["w_in"], f)[0]
    cols = []
    for h in range(8):
        c = []
        for blk in (0, 1024):
            for m in range(2):
                c.extend(range(blk + m * 512 + h * 64, blk + m * 512 + h * 64 + 64))
        c.extend(range(2048 + h * 128, 2048 + h * 128 + 128))
        cols.append(c)
    WhR = np.concatenate([_arr_kc(w_in[:, c], 384) for c in cols], axis=0)
    WgR = _arr_kc(w_in[:, 3072:], 512)
    WbrR = np.concatenate([_arr_kc(np.asarray(inp[k], f)[0], 512) for k in ("w_branch_attn", "w_branch_sg", "w_out")], axis=0)
    vecs = np.zeros((8, 1024), f)
    vecs[0] = np.asarray(inp["norm1_g"], f)[0]
    vecs[1] = np.asarray(inp["sg_ln_g"], f)[0]
    vecs[2] = np.asarray(inp["sg_ln_b"], f)[0]
    vecs[3] = np.asarray(inp["norm2_g"], f)[0]
    vecs[4] = np.asarray(inp["final_g"], f)
    vecs[5, :128] = np.asarray(inp["da_subln_g"], f)[0]
    vecs[6, 0:64] = np.asarray(inp["lambda_q1"], f)[0]
    vecs[6, 64:128] = np.asarray(inp["lambda_k1"], f)[0]
    vecs[6, 128:192] = np.asarray(inp["lambda_q2"], f)[0]
    vecs[6, 192:256] = np.asarray(inp["lambda_k2"], f)[0]
    sgw = np.asarray(inp["sg_w"], f)[0]
    sgwT = np.ascontiguousarray(sgw.transpose(2, 0, 1)).reshape(128, 8 * 128)
    sgb = np.asarray(inp["sg_b"], f)[0].reshape(1, 8 * 128)
    wqR = _arr_kc(np.asarray(inp["peer_w_query"], f)[0], 512)
    sk = np.asarray(inp["peer_subkeys"], f)[0]
    skT = np.ascontiguousarray(sk.transpose(2, 0, 1)).reshape(128, 256)
    U = np.asarray(inp["peer_u"], f)[0]
    UTR = np.ascontiguousarray(U.reshape(128, 128, 8, 128).transpose(0, 3, 2, 1)).reshape(128, 128, 1024)
    VR = np.asarray(inp["peer_v"], f)[0].reshape(128, 128, 1024)
    iotas = np.zeros((128, 2048 + 128), f)
    iotas[:, :2048] = (np.arange(2048) % 16)[None, :]
    iotas[:, 2048:] = np.arange(128)[None, :]
    return {"WhR": WhR, "WgR": WgR, "WbrR": WbrR, "vecs": vecs, "sgwT": sgwT, "sgb": sgb,
            "ident": np.eye(128, dtype=f), "wqR": wqR, "skT": skT, "UTR": UTR, "VR": VR, "iotas": iotas}


_CACHE = {}


def kernel(**inputs):
    x = np.asarray(inputs["x"], np.float32)
    shared = prep_shared(inputs)
    if "nc" not in _CACHE:
        nc = bass.Bass("TRN2", target_bir_lowering=False)
        K(nc).build()
        _CACHE["nc"] = nc
    nc = _CACHE["nc"]
    in_maps = [dict(shared, x=np.ascontiguousarray(x[b])) for b in range(8)]
    res = run_bass_kernel_spmd(nc, in_maps, core_ids=list(range(8)))
    return np.stack([np.asarray(r["out"], np.float32) for r in res.results], axis=0)
```
